# Optimizing a Trainium2 kernel written in Bass

```python
import jax, jax.numpy as jnp
from jax import lax
import numpy as np

D_MODEL = 1024
BATCH = 8
SEQ = 8192
DEPTH = 2

N_META = 16
N_A_LAYERS = DEPTH // 2
N_B_LAYERS = DEPTH - N_A_LAYERS
POOL_WINDOWS = (2, 4, 8, 16)
POOL_GROUP = D_MODEL // len(POOL_WINDOWS)
SB_HEADS = 16
SB_HEAD_DIM = D_MODEL // SB_HEADS
Q_BLOCK = 128
PEER_HEADS = 8
PEER_N_KEYS = 128
PEER_N_EXPERTS = PEER_N_KEYS * PEER_N_KEYS
PEER_TOPK = 16
PEER_QUERY_DIM = 256
PEER_HALF = PEER_QUERY_DIM // 2
PEER_CHUNK = 16
EPS = 1e-6

kernel_name = "yoco_pool_stickbreak_peer"


def rmsnorm(x, g):
    xf = x.astype(jnp.float32)
    y = xf * lax.rsqrt(jnp.mean(xf * xf, axis=-1, keepdims=True) + EPS)
    return (y * g.astype(jnp.float32)).astype(x.dtype)


def pool_mixer(h, w_pool, scale):
    L = h.shape[1]
    hf = h.astype(jnp.float32)
    pos = jnp.arange(L)
    outs = []
    for g, w in enumerate(POOL_WINDOWS):
        hg = hf[..., g * POOL_GROUP:(g + 1) * POOL_GROUP]
        c = jnp.cumsum(hg, axis=1)
        c_prev = jnp.pad(c, ((0, 0), (w, 0), (0, 0)))[:, :L]
        cnt = jnp.minimum(pos + 1, w).astype(jnp.float32)[None, :, None]
        pooled = ((c - c_prev) / cnt - hg).astype(h.dtype)
        outs.append(jnp.einsum('bsc,cd->bsd', pooled, w_pool[g]))
    return jnp.concatenate(outs, axis=-1) * scale


def stick_breaking(q, k, v):
    B, L, H, dh = q.shape
    pad = (-L) % Q_BLOCK
    Lp = L + pad
    nblk = Lp // Q_BLOCK
    pw = ((0, 0), (pad, 0), (0, 0), (0, 0))
    qb = jnp.pad(q, pw).reshape(B, nblk, Q_BLOCK, H, dh).transpose(1, 0, 3, 2, 4)
    kp = jnp.pad(k, pw).transpose(0, 2, 1, 3)
    vp = jnp.pad(v, pw).transpose(0, 2, 1, 3)
    key_pos = jnp.arange(Lp)
    key_real = key_pos >= pad
    scale = SB_HEAD_DIM ** -0.5

    def block(args):
        q_blk, i = args
        q_pos = i * Q_BLOCK + jnp.arange(Q_BLOCK)
        z = jnp.einsum('bhqd,bhkd->bhqk', q_blk, kp,
                       preferred_element_type=jnp.float32) * scale
        valid = (key_pos[None, :] < q_pos[:, None]) & key_real[None, :]
        log_keep = jnp.where(valid, jax.nn.log_sigmoid(-z), 0.0)
        after = lax.cumsum(log_keep, axis=3, reverse=True) - log_keep
        log_w = jax.nn.log_sigmoid(z) + after
        wgt = jnp.where(valid, jnp.exp(log_w), 0.0)
        return jnp.einsum('bhqk,bhkd->bqhd', wgt.astype(v.dtype), vp)

    out = lax.map(block, (qb, jnp.arange(nblk)))
    out = out.transpose(1, 0, 2, 3, 4).reshape(B, Lp, H * dh)
    return out[:, pad:]


def peer(h, w_query, sub_keys, u, v):
    B, L, D = h.shape
    nchunk = L // PEER_CHUNK
    hc = h.reshape(B, nchunk, PEER_CHUNK, D).transpose(1, 0, 2, 3)

    def chunk(hx):
        q = jnp.einsum('btd,dq->btq', hx, w_query).reshape(
            B, PEER_CHUNK, PEER_HEADS, 2, PEER_HALF)
        s = jnp.einsum('bthpc,hpnc->bthpn', q, sub_keys,
                       preferred_element_type=jnp.float32)
        top_s, top_i = lax.top_k(s, PEER_TOPK)
        cand_s = (top_s[..., 0, :, None] + top_s[..., 1, None, :]).reshape(
            B, PEER_CHUNK, PEER_HEADS, PEER_TOPK * PEER_TOPK)
        cand_i = (top_i[..., 0, :, None] * PEER_N_KEYS + top_i[..., 1, None, :]).reshape(
            B, PEER_CHUNK, PEER_HEADS, PEER_TOPK * PEER_TOPK)
        best_s, best_pos = lax.top_k(cand_s, PEER_TOPK)
        expert = jnp.take_along_axis(cand_i, best_pos, axis=-1)
        gate = jax.nn.softmax(best_s, axis=-1)
        u_sel = jnp.take(u, expert, axis=0)
        v_sel = jnp.take(v, expert, axis=0)
        act = jax.nn.gelu(jnp.einsum('btd,bthkd->bthk', hx, u_sel,
                                     preferred_element_type=jnp.float32), approximate=False)
        return jnp.einsum('bthk,bthkd->btd', (gate * act).astype(h.dtype), v_sel)

    y = lax.map(chunk, hc)
    return y.transpose(1, 0, 2, 3).reshape(B, L, D)


def setup_inputs(seed: int = 0) -> dict:
    key = jax.random.key(seed)
    ks = jax.random.split(key, 16)
    f = jnp.float32
    D = D_MODEL
    nrm = lambda k, shape, s: jax.random.normal(k, shape, f) * s
    return {
        "x": jax.random.normal(ks[0], (BATCH, SEQ, D), f),
        "meta_tokens": nrm(ks[1], (N_META, D), 1.0),
        "norm_mix": 1.0 + nrm(ks[2], (DEPTH, D), 0.02),
        "norm_ffn": 1.0 + nrm(ks[3], (DEPTH, D), 0.02),
        "pool_w": nrm(ks[4], (N_A_LAYERS, len(POOL_WINDOWS), POOL_GROUP, POOL_GROUP), POOL_GROUP ** -0.5),
        "pool_scale": 0.5 + nrm(ks[5], (N_A_LAYERS, D), 0.05),
        "kv_norm": 1.0 + nrm(ks[6], (D,), 0.02),
        "w_kv": nrm(ks[7], (D, 2 * D), D ** -0.5),
        "k_norm": 1.0 + nrm(ks[8], (SB_HEAD_DIM,), 0.02),
        "w_q": nrm(ks[9], (N_B_LAYERS, D, D), D ** -0.5),
        "q_norm": 1.0 + nrm(ks[10], (N_B_LAYERS, SB_HEAD_DIM), 0.02),
        "w_o": nrm(ks[11], (N_B_LAYERS, D, D), D ** -0.5),
        "peer_wq": nrm(ks[12], (DEPTH, D, PEER_HEADS * PEER_QUERY_DIM), D ** -0.5),
        "peer_keys": nrm(ks[13], (DEPTH, PEER_HEADS, 2, PEER_N_KEYS, PEER_HALF), PEER_HALF ** -0.5),
        "peer_u": nrm(ks[14], (DEPTH, PEER_N_EXPERTS, D), D ** -0.5),
        "peer_v": nrm(ks[15], (DEPTH, PEER_N_EXPERTS, D), 0.25),
    }


def reference(x, meta_tokens, norm_mix, norm_ffn, pool_w, pool_scale, kv_norm, w_kv, k_norm,
              w_q, q_norm, w_o, peer_wq, peer_keys, peer_u, peer_v):
    B = x.shape[0]
    meta = jnp.broadcast_to(meta_tokens[None].astype(x.dtype), (B, N_META, D_MODEL))
    h = jnp.concatenate([meta, x], axis=1)
    L = h.shape[1]
    k_sh = None
    v_sh = None
    for layer in range(DEPTH):
        hn = rmsnorm(h, norm_mix[layer])
        if layer < N_A_LAYERS:
            mix = pool_mixer(hn, pool_w[layer], pool_scale[layer])
        else:
            j = layer - N_A_LAYERS
            q = jnp.einsum('bld,de->ble', hn, w_q[j]).reshape(B, L, SB_HEADS, SB_HEAD_DIM)
            q = rmsnorm(q, q_norm[j])
            mix = jnp.einsum('ble,ed->bld', stick_breaking(q, k_sh, v_sh), w_o[j])
        h = h + mix
        h = h + peer(rmsnorm(h, norm_ffn[layer]), peer_wq[layer], peer_keys[layer],
                     peer_u[layer], peer_v[layer])
        if layer == N_A_LAYERS - 1:
            kv = jnp.einsum('bld,de->ble', rmsnorm(h, kv_norm), w_kv)
            k_sh = rmsnorm(kv[..., :D_MODEL].reshape(B, L, SB_HEADS, SB_HEAD_DIM), k_norm)
            v_sh = kv[..., D_MODEL:].reshape(B, L, SB_HEADS, SB_HEAD_DIM)
    return h[:, N_META:]
```

```python
import numpy as np
from contextlib import ExitStack
import concourse.bass as bass
import concourse.mybir as mybir
from concourse.alu_op_type import AluOpType as ALU
from concourse.bass_utils import run_bass_kernel_spmd

F32 = mybir.dt.float32
BF16 = mybir.dt.bfloat16
U32 = mybir.dt.uint32
I32 = mybir.dt.int32
AF = mybir.ActivationFunctionType
AX = mybir.AxisListType

D = 1024
EPS = 1e-6
NEG = -1.0e30
NB = 10
COMPUTE = ('pe', 'act', 'dve', 'pool')


class Buf:
    __slots__ = ('t', 'w', 'fw', 'r', 'name')

    def __init__(self, t, name=''):
        self.t = t
        self.w = {}
        self.fw = {}
        self.r = {}
        self.name = name


class K:
    def __init__(self, nc, es):
        self.nc = nc
        self.es = es
        self.engs = {'pe': nc.tensor, 'act': nc.scalar, 'dve': nc.vector,
                     'pool': nc.gpsimd, 'sp': nc.sync}
        self.semh = {}
        self.cnt = {}
        for e in COMPUTE:
            self.semh[e] = es.enter_context(nc.semaphore('sem_' + e))
            self.cnt[e] = 0
        self.seen = {e: {} for e in self.engs}
        self.ninst = 0

    def sb(self, es, name, shape, dt):
        self.nalloc = getattr(self, 'nalloc', 0) + 1
        return Buf(es.enter_context(self.nc.sbuf_tensor("s%d_%s" % (self.nalloc, name), list(shape), dt)), name)

    def _dsem(self, key):
        if key not in self.semh:
            self.semh[key] = self.es.enter_context(self.nc.semaphore('d_' + key))
            self.cnt[key] = 0
        return self.semh[key]

    def _wait(self, e, deps):
        seen = self.seen[e]
        for key, val in deps.items():
            if val <= 0 or seen.get(key, 0) >= val:
                continue
            self.engs[e].wait_ge(self.semh[key], val)
            seen[key] = val
            self.ninst += 1

    @staticmethod
    def _merge(d, s):
        for k_, v in s.items():
            if d.get(k_, 0) < v:
                d[k_] = v

    def _deps(self, r, w, pw):
        deps = {}
        for b in r:
            self._merge(deps, b.w)
        for b in w:
            self._merge(deps, b.w)
            self._merge(deps, b.r)
        for b in pw:
            self._merge(deps, b.fw)
            self._merge(deps, b.r)
        return deps

    def _mark(self, key, val, r, w, pw):
        for b in r:
            if b.r.get(key, 0) < val:
                b.r[key] = val
        for b in w:
            b.w = {key: val}
            b.fw = {key: val}
            b.r = {}
        for b in pw:
            if b.w.get(key, 0) < val:
                b.w[key] = val

    rec = None

    def replay(self, rec, n, ndve=None):
        saved, self.rec = self.rec, None
        nd = 0
        for _ in range(min(n, len(rec))):
            kind, args, kw = rec[0]
            if ndve is not None and kind == 'op' and args[0] == 'dve':
                if nd >= ndve:
                    break
                nd += 1
            rec.pop(0)
            (self.op if kind == 'op' else self.dma)(*args, **kw)
        self.rec = saved

    def op(self, e, fn, r=(), w=(), pw=()):
        if self.rec is not None:
            self.rec.append(('op', (e, fn), dict(r=r, w=w, pw=pw)))
            return
        self._wait(e, self._deps(r, w, pw))
        inst = fn(self.engs[e])
        self.cnt[e] += 1
        inst.then_inc(self.semh[e], 1)
        self.ninst += 1
        self._mark(e, self.cnt[e], r, w, pw)

    def dma(self, q, key, fn, r=(), w=(), pw=()):
        if self.rec is not None:
            self.rec.append(('dma', (q, key, fn), dict(r=r, w=w, pw=pw)))
            return
        self._dsem(key)
        deps = self._deps(r, w, pw)
        if self.cnt[key] > 0:
            deps[key] = max(deps.get(key, 0), self.cnt[key])
        self._wait(q, deps)
        inst = fn(self.engs[q])
        self.cnt[key] += 16
        inst.then_inc(self.semh[key], 16)
        self.ninst += 1
        self._mark(key, self.cnt[key], r, w, pw)

    def barrier(self):
        alld = {key: v for key, v in self.cnt.items() if v > 0}
        for e in self.engs:
            self._wait(e, alld)


def bcast_rows(ap2d_row, nparts):
    return ap2d_row.partition_broadcast(nparts)


def build(NT, dbg=False):
    nc = bass.Bass("TRN2", target_bir_lowering=False)
    Lp = NT * 128
    NQ = NT - 1
    din = lambda name, shape, dt=F32: nc.dram_tensor(name, list(shape), dt, kind="ExternalInput").ap()
    xpad = din("xpad", [Lp, D])
    gains_d = din("gains", [6, D])
    hnorm_d = din("hnorm", [2, 64])
    poolw_d = din("poolw", [4, 256, 256])
    bands_d = din("bands", [128, 12, 128])
    wq_d = din("wq", [2, D, 2048])
    keysT_d = din("keysT", [2, 128, 2048])
    wkv_d = din("wkv", [D, 2048])
    wqa_d = din("wqa", [D, D])
    wo_d = din("wo", [D, D])
    pu_d = [din("pu%d" % l, [16384, D]) for l in range(2)]
    pv_d = [din("pv%d" % l, [16384, D]) for l in range(2)]
    cst_d = din("cst", [128, 4, 128])
    rowm_d = din("rowm", [128, 1])
    iota_d = din("iota", [128, 2048])
    out_d = nc.dram_tensor("out", [NQ * 128, D], F32, kind="ExternalOutput").ap()
    skind = "ExternalOutput" if dbg else "Internal"
    H2 = nc.dram_tensor("H2", [Lp, D], F32, kind=skind).ap()
    KT = nc.dram_tensor("KT", [16, 64, Lp], BF16, kind=skind).ap()
    QT = nc.dram_tensor("QT", [16, 64, Lp], BF16, kind=skind).ap()
    VV = nc.dram_tensor("VV", [Lp, D], BF16, kind=skind).ap()
    AT = nc.dram_tensor("AT", [D, Lp], BF16, kind=skind).ap()
    UVb = [nc.dram_tensor("UVb%d" % l, [16384, 2 * D], BF16, kind="Internal").ap() for l in range(2)]

    with ExitStack() as es:
        k = K(nc, es)
        ps = [Buf(es.enter_context(nc.psum_tensor("ps%d" % i, [128, 512], F32)), "ps%d" % i)
              for i in range(8)]
        ident_b = k.sb(es, "ident_b", [128, 128], BF16)
        ident_f = k.sb(es, "ident_f", [128, 128], F32)
        trim = k.sb(es, "trim", [128, 128], BF16)
        negtri = k.sb(es, "negtri", [128, 128], BF16)
        negones = k.sb(es, "negones", [128, 128], BF16)
        rowm = k.sb(es, "rowm", [128, 1], F32)
        epsb = k.sb(es, "epsb", [128, 1], F32)
        k.dma('pool', 'c0', lambda e: e.dma_start(out=ident_b.t[:, :], in_=cst_d[:, 0, :]), w=[ident_b])
        k.dma('sp', 'c1', lambda e: e.dma_start(out=ident_f.t[:, :], in_=cst_d[:, 0, :]), w=[ident_f])
        k.dma('pool', 'c2', lambda e: e.dma_start(out=trim.t[:, :], in_=cst_d[:, 1, :]), w=[trim])
        k.dma('pool', 'c3', lambda e: e.dma_start(out=negtri.t[:, :], in_=cst_d[:, 2, :]), w=[negtri])
        k.dma('pool', 'c4', lambda e: e.dma_start(out=negones.t[:, :], in_=cst_d[:, 3, :]), w=[negones])
        k.dma('sp', 'c5', lambda e: e.dma_start(out=rowm.t[:, :], in_=rowm_d[:, :]), w=[rowm])
        k.op('dve', lambda e: e.memset(epsb.t[:, :], EPS), w=[epsb])

        uvb = [Buf(None, "uvb%d" % l) for l in range(2)]

        def convert_tables(l):
            for which, src in ((0, pu_d[l]), (1, pv_d[l])):
                for rr in range(4):
                    k.dma('pool', 'cv%d' % ((which * 4 + rr) % 4), lambda e, l=l, which=which, src=src, rr=rr: e.dma_start(
                        out=UVb[l][rr * 4096:(rr + 1) * 4096, which * D:(which + 1) * D],
                        in_=src[rr * 4096:(rr + 1) * 4096, :]), pw=[uvb[l]])

        convert_tables(0)

        def rstd_of(src, n_feat, ms, rstd, junk, src_ap=None):
            sap = src.t[:, :] if src_ap is None else src_ap
            k.op('act', lambda e: e.activation(out=junk.t[:, :], in_=sap, func=AF.Square,
                                               scale=float(n_feat) ** -0.5, accum_out=ms.t[:, :]),
                 r=[src], w=[junk, ms])
            k.op('act', lambda e: e.activation(out=ms.t[:, :], in_=ms.t[:, :], func=AF.Ln,
                                               bias=epsb.t[:, :], scale=1.0), r=[epsb], w=[ms])
            k.op('act', lambda e: e.activation(out=rstd.t[:, :], in_=ms.t[:, :], func=AF.Exp, scale=-0.5),
                 r=[ms], w=[rstd])

        neghalf = k.sb(es, "neghalf", [128, 1], F32)
        ebase = k.sb(es, "ebase", [128, 128], F32)
        k.op('dve', lambda e: e.memset(neghalf.t[:, :], -0.5), w=[neghalf])
        k.op('dve', lambda e: e.memset(ebase.t[:, :], float(np.e)), w=[ebase])

        def rstd_pow(src, n_feat, ms, rstd, junk):
            k.op('act', lambda e: e.activation(out=junk.t[:, :], in_=src.t[:, :], func=AF.Square,
                                               scale=float(n_feat) ** -0.5, accum_out=ms.t[:, :]),
                 r=[src], w=[junk, ms])
            k.op('pool', lambda e: e.tensor_scalar(out=ms.t[:, :], in0=ms.t[:, :], scalar1=EPS, scalar2=None,
                                                   op0=ALU.add), w=[ms])
            k.op('pool', lambda e: e.tensor_tensor(out=rstd.t[:, :], in0=ms.t[:, :], in1=neghalf.t[:, :], op=ALU.pow),
                 r=[ms, neghalf], w=[rstd])

        class PeerState:
            pass

        def peer_alloc(pes):
            st = PeerState()
            st.iota = k.sb(pes, "iota", [128, 2048], F32)
            k.dma('sp', 'iota', lambda e: e.dma_start(out=st.iota.t[:, :], in_=iota_d[:, :]), w=[st.iota])
            st.wq = k.sb(pes, "wq_sb", [128, 8, 2048], BF16)
            st.keysT = k.sb(pes, "keysT_sb", [128, 2048], BF16)
            st.junk = k.sb(pes, "pjunk", [128, D], F32)
            st.ms = k.sb(pes, "pms", [128, 1], F32)
            st.rstd = k.sb(pes, "prstd", [128, 1], F32)
            st.hnb = [k.sb(pes, "hnb%d" % i, [128, D], BF16) for i in range(2)]
            st.hnT = k.sb(pes, "hnT", [128, D], BF16)
            st.qT = k.sb(pes, "qT", [128, 2048], BF16)
            st.scr = k.sb(pes, "scr", [128, 2048], F32)
            st.top = k.sb(pes, "top", [128, 256], F32)
            st.idx = k.sb(pes, "idx", [128, 256], U32)
            st.idxf = k.sb(pes, "idxf", [128, 256], F32)
            st.cand = k.sb(pes, "cand", [128, 2048], F32)
            st.best = k.sb(pes, "best", [128, 128], F32)
            st.pos = k.sb(pes, "pos", [128, 128], U32)
            st.posi = k.sb(pes, "posi", [128, 128], U32)
            st.posj = k.sb(pes, "posj", [128, 128], U32)
            st.pif = k.sb(pes, "pif", [128, 128], F32)
            st.pjf = k.sb(pes, "pjf", [128, 128], F32)
            st.oh = st.scr
            st.ea = k.sb(pes, "ea", [128, 128], F32)
            st.eb = k.sb(pes, "eb", [128, 128], F32)
            st.ef = k.sb(pes, "ef", [128, 128], F32)
            st.ssum = k.sb(pes, "ssum", [128, 8], F32)
            st.gate = [k.sb(pes, "gate%d" % i, [128, 128], F32) for i in range(2)]
            st.eidx = [k.sb(pes, "eidx%d" % i, [128, 128], U32) for i in range(2)]
            st.act = [k.sb(pes, "pact%d" % i, [128, 1], F32) for i in range(4)]
            st.gel = [k.sb(pes, "pgel%d" % i, [128, 1], F32) for i in range(4)]
            st.prod = [k.sb(pes, "prod%d" % i, [128, D], BF16) for i in range(3)]
            st.slots = [k.sb(pes, "gs%d" % i, [128, 2 * D], BF16) for i in range(NB)]
            st.wv = [k.sb(pes, "pwv%d" % i, [128, 1], F32) for i in range(4)]
            st.vs = [k.sb(pes, "pvs%d" % i, [128, D], BF16) for i in range(3)]
            st.gcount = 0
            return st

        def peer_load_weights(st, l):
            for c in range(8):
                k.dma('pool', 'wq%d' % (c % 2),
                      lambda e, c=c: e.dma_start(out=st.wq.t[:, c, :], in_=wq_d[l, c * 128:(c + 1) * 128, :]),
                      pw=[st.wq])
            k.dma('pool', 'keysT', lambda e: e.dma_start(out=st.keysT.t[:, :], in_=keysT_d[l, :, :]),
                  w=[st.keysT])

        def peer_prep(st, h, gain_ap, gain_buf, par):
            hnb, gate, eidx = st.hnb[par], st.gate[par], st.eidx[par]
            rstd_pow(h, D, st.ms, st.rstd, st.junk)
            k.op('dve', lambda e: e.scalar_tensor_tensor(out=hnb.t[:, :], in0=h.t[:, :], scalar=st.rstd.t[:, :],
                                                         in1=gain_ap, op0=ALU.mult, op1=ALU.mult),
                 r=[h, st.rstd, gain_buf], w=[hnb])
            pT = ps[0]
            pTb = pT.t[:, :].bitcast(BF16)
            for c in range(8):
                k.op('pe', lambda e, c=c: e.transpose(out=pTb[:, c * 128:(c + 1) * 128],
                                                      in_=hnb.t[:, c * 128:(c + 1) * 128],
                                                      identity=ident_b.t[:, :]),
                     r=[hnb, ident_b], **({'w': [pT]} if c == 0 else {'pw': [pT]}))
            k.op('act', lambda e: e.activation(out=st.hnT.t[:, :], in_=pTb, func=AF.Copy), r=[pT], w=[st.hnT])
            for bnk in range(4):
                pq = ps[1 + bnk % 2]
                for jj in range(4):
                    j = bnk * 4 + jj
                    for c in range(8):
                        first = (jj == 0 and c == 0)
                        k.op('pe', lambda e, j=j, jj=jj, c=c, pq=pq: e.matmul(
                            pq.t[:, jj * 128:(jj + 1) * 128],
                            lhsT=st.wq.t[:, c, j * 128:(j + 1) * 128],
                            rhs=st.hnT.t[:, c * 128:(c + 1) * 128],
                            start=(c == 0), stop=(c == 7)),
                             r=[st.wq, st.hnT], **({'w': [pq]} if first else {'pw': [pq]}))
                k.op('act', lambda e, bnk=bnk, pq=pq: e.activation(
                    out=st.qT.t[:, bnk * 512:(bnk + 1) * 512], in_=pq.t[:, :], func=AF.Copy),
                     r=[pq], **({'w': [st.qT]} if bnk == 0 else {'pw': [st.qT]}))

            def scores(bnk):
                pq = ps[1 + bnk % 2]
                for jj in range(4):
                    g = bnk * 4 + jj
                    k.op('pe', lambda e, g=g, jj=jj, pq=pq: e.matmul(
                        pq.t[:, jj * 128:(jj + 1) * 128],
                        lhsT=st.qT.t[:, g * 128:(g + 1) * 128],
                        rhs=st.keysT.t[:, g * 128:(g + 1) * 128],
                        start=True, stop=True),
                         r=[st.qT, st.keysT], **({'w': [pq]} if jj == 0 else {'pw': [pq]}))

            def topk1(bnk):
                pq = ps[1 + bnk % 2]
                for g in range(bnk * 4, bnk * 4 + 4):
                    sg = pq.t[:, (g % 4) * 128:(g % 4 + 1) * 128]
                    t0 = st.top.t[:, g * 16:g * 16 + 8]
                    t1 = st.top.t[:, g * 16 + 8:g * 16 + 16]
                    scr = st.scr.t[:, g * 128:(g + 1) * 128]
                    k.op('dve', lambda e, sg=sg, t0=t0: e.max(out=t0, in_=sg), r=[pq], pw=[st.top])
                    k.op('dve', lambda e, sg=sg, t0=t0, scr=scr: e.match_replace(
                        out=scr, in_to_replace=t0, in_values=sg, imm_value=NEG), r=[pq, st.top], pw=[st.scr])
                    k.op('dve', lambda e, t1=t1, scr=scr: e.max(out=t1, in_=scr), r=[st.scr], pw=[st.top])
                    k.op('dve', lambda e, g=g, sg=sg, t0=t0: e.max_index(
                        out=st.idx.t[:, g * 16:g * 16 + 8], in_max=t0, in_values=sg), r=[pq, st.top], pw=[st.idx])
                    k.op('dve', lambda e, g=g, sg=sg, t1=t1: e.max_index(
                        out=st.idx.t[:, g * 16 + 8:g * 16 + 16], in_max=t1, in_values=sg), r=[pq, st.top], pw=[st.idx])

            scores(0)
            scores(1)
            topk1(0)
            scores(2)
            topk1(1)
            scores(3)
            topk1(2)
            topk1(3)
            k.op('dve', lambda e: e.tensor_copy(out=st.idxf.t[:, :], in_=st.idx.t[:, :]), r=[st.idx], w=[st.idxf])
            top4 = st.top.t[:, :].rearrange("p (h two i) -> p h two i", h=8, two=2)
            in0 = top4[:, :, 0, :].unsqueeze(3).to_broadcast([128, 8, 16, 16])
            in1 = top4[:, :, 1, :].unsqueeze(2).to_broadcast([128, 8, 16, 16])
            cand4 = st.cand.t[:, :].rearrange("p (h i j) -> p h i j", h=8, i=16)
            k.op('dve', lambda e: e.tensor_tensor(out=cand4, in0=in0, in1=in1, op=ALU.add), r=[st.top], w=[st.cand])
            for hh in range(8):
                cg = st.cand.t[:, hh * 256:(hh + 1) * 256]
                b0 = st.best.t[:, hh * 16:hh * 16 + 8]
                b1 = st.best.t[:, hh * 16 + 8:hh * 16 + 16]
                scr = st.scr.t[:, hh * 256:(hh + 1) * 256]
                k.op('dve', lambda e, cg=cg, b0=b0: e.max(out=b0, in_=cg), r=[st.cand], pw=[st.best])
                k.op('dve', lambda e, cg=cg, b0=b0, scr=scr: e.match_replace(
                    out=scr, in_to_replace=b0, in_values=cg, imm_value=NEG), r=[st.cand, st.best], pw=[st.scr])
                k.op('dve', lambda e, b1=b1, scr=scr: e.max(out=b1, in_=scr), r=[st.scr], pw=[st.best])
                k.op('dve', lambda e, hh=hh, cg=cg, b0=b0: e.max_index(
                    out=st.pos.t[:, hh * 16:hh * 16 + 8], in_max=b0, in_values=cg), r=[st.cand, st.best], pw=[st.pos])
                k.op('dve', lambda e, hh=hh, cg=cg, b1=b1: e.max_index(
                    out=st.pos.t[:, hh * 16 + 8:hh * 16 + 16], in_max=b1, in_values=cg), r=[st.cand, st.best], pw=[st.pos])
            best3 = st.best.t[:, :].rearrange("p (h k) -> p h k", h=8)
            gate3 = gate.t[:, :].rearrange("p (h k) -> p h k", h=8)
            k.op('dve', lambda e: e.tensor_tensor(out=gate3, in0=best3,
                                                  in1=best3[:, :, 0:1].to_broadcast([128, 8, 16]), op=ALU.subtract),
                 r=[st.best], w=[gate])
            k.op('pool', lambda e: e.tensor_tensor(out=gate.t[:, :], in0=ebase.t[:, :], in1=gate.t[:, :], op=ALU.pow),
                 r=[ebase], w=[gate])
            k.op('dve', lambda e: e.tensor_reduce(out=st.ssum.t[:, :], in_=gate3, axis=AX.X, op=ALU.add),
                 r=[gate], w=[st.ssum])
            k.op('dve', lambda e: e.reciprocal(out=st.ssum.t[:, :], in_=st.ssum.t[:, :]), r=[st.ssum], w=[st.ssum])
            k.op('dve', lambda e: e.tensor_tensor(out=gate3, in0=gate3,
                                                  in1=st.ssum.t[:, :].unsqueeze(2).to_broadcast([128, 8, 16]),
                                                  op=ALU.mult), r=[gate, st.ssum], w=[gate])
            k.op('dve', lambda e: e.tensor_scalar(out=st.posi.t[:, :], in0=st.pos.t[:, :], scalar1=4, scalar2=None,
                                                  op0=ALU.logical_shift_right), r=[st.pos], w=[st.posi])
            k.op('dve', lambda e: e.tensor_scalar(out=st.posj.t[:, :], in0=st.pos.t[:, :], scalar1=15, scalar2=None,
                                                  op0=ALU.bitwise_and), r=[st.pos], w=[st.posj])
            k.op('dve', lambda e: e.tensor_copy(out=st.pif.t[:, :], in_=st.posi.t[:, :]), r=[st.posi], w=[st.pif])
            k.op('dve', lambda e: e.tensor_copy(out=st.pjf.t[:, :], in_=st.posj.t[:, :]), r=[st.posj], w=[st.pjf])
            iota4 = st.iota.t[:, :].rearrange("p (h k i) -> p h k i", h=8, k=16)
            oh4 = st.oh.t[:, :].rearrange("p (h k i) -> p h k i", h=8, k=16)
            idx4 = st.idxf.t[:, :].rearrange("p (h two i) -> p h two i", h=8, two=2)
            for which, pf, dst in ((0, st.pif, st.ea), (1, st.pjf, st.eb)):
                pf4 = pf.t[:, :].rearrange("p (h k) -> p h k", h=8).unsqueeze(3).to_broadcast([128, 8, 16, 16])
                k.op('dve', lambda e, pf4=pf4: e.tensor_tensor(out=oh4, in0=iota4, in1=pf4, op=ALU.is_equal),
                     r=[st.iota, pf], w=[st.oh])
                ix = idx4[:, :, which, :].unsqueeze(2).to_broadcast([128, 8, 16, 16])
                k.op('dve', lambda e, ix=ix: e.tensor_tensor(out=oh4, in0=oh4, in1=ix, op=ALU.mult),
                     r=[st.oh, st.idxf], w=[st.oh])
                k.op('dve', lambda e, dst=dst: e.tensor_reduce(
                    out=dst.t[:, :], in_=st.oh.t[:, :].rearrange("p (a i) -> p a i", i=16), axis=AX.X, op=ALU.add),
                     r=[st.oh], w=[dst])
            k.op('dve', lambda e: e.scalar_tensor_tensor(out=st.ef.t[:, :], in0=st.ea.t[:, :], scalar=128.0,
                                                         in1=st.eb.t[:, :], op0=ALU.mult, op1=ALU.add),
                 r=[st.ea, st.eb], w=[st.ef])
            k.op('dve', lambda e: e.tensor_copy(out=eidx.t[:, :], in_=st.ef.t[:, :]), r=[st.ef], w=[eidx])

        class Pipe:
            def __init__(self):
                self.n = 0
                self.sched = {}

            def add(self, step, fn):
                self.sched.setdefault(step, []).append(fn)

            def run(self, step):
                for fn in self.sched.pop(step, []):
                    fn()

            def flush(self):
                for step in sorted(self.sched):
                    self.run(step)

        def peer_gather(pipe, st, l, h, par, ybanks, rec=None, fin=None):
            hnb, gate, eidx = st.hnb[par], st.gate[par], st.eidx[par]
            pa, pb = ybanks
            slot_of = {}

            def G(s_):
                slot = st.slots[st.gcount % NB]
                st.gcount += 1
                slot_of[s_] = slot
                k.dma('pool', 'g_' + slot.name, lambda e: e.indirect_dma_start(
                    out=slot.t[:, :], out_offset=None, in_=UVb[l][:, :],
                    in_offset=bass.IndirectOffsetOnAxis(ap=eidx.t[:, s_:s_ + 1], axis=0)),
                      r=[eidx, uvb[l]], w=[slot])

            def Dt(s_):
                slot = slot_of[s_]
                pr = st.prod[s_ % 3]
                k.op('dve', lambda e: e.tensor_tensor(out=pr.t[:, :], in0=slot.t[:, 0:D], in1=hnb.t[:, :], op=ALU.mult),
                     r=[slot, hnb], w=[pr])

            def P(s_):
                pr = st.prod[s_ % 3]
                pd = ps[3 + s_ % 2]
                for c in range(8):
                    k.op('pe', lambda e, c=c: e.matmul(
                        pd.t[:, 0:128], lhsT=ident_b.t[:, :], rhs=pr.t[:, c * 128:(c + 1) * 128],
                        start=(c == 0), stop=(c == 7)),
                         r=[ident_b, pr], **({'w': [pd]} if c == 0 else {'pw': [pd]}))

            def R(s_):
                pd = ps[3 + s_ % 2]
                a_ = st.act[s_ % 4]
                k.op('dve', lambda e: e.tensor_reduce(out=a_.t[:, :], in_=pd.t[:, 0:128], axis=AX.X, op=ALU.add),
                     r=[pd], w=[a_])

            def E(s_):
                a_, g_ = st.act[s_ % 4], st.gel[s_ % 4]
                k.op('act', lambda e: e.activation(out=g_.t[:, :], in_=a_.t[:, :], func=AF.Gelu), r=[a_], w=[g_])

            def X(s_):
                g_, w_ = st.gel[s_ % 4], st.wv[s_ % 4]
                k.op('dve', lambda e: e.tensor_tensor(out=w_.t[:, :], in0=g_.t[:, :], in1=gate.t[:, s_:s_ + 1],
                                                      op=ALU.mult), r=[g_, gate], w=[w_])

            def W(s_):
                slot, w_, vs = slot_of[s_], st.wv[s_ % 4], st.vs[s_ % 3]
                k.op('act', lambda e: e.activation(out=vs.t[:, :], in_=slot.t[:, D:2 * D], func=AF.Copy,
                                                   scale=w_.t[:, :]), r=[slot, w_], w=[vs])

            def M(s_):
                vs = st.vs[s_ % 3]
                for half, pb_ in ((0, pa), (1, pb)):
                    k.op('pe', lambda e, half=half, pb_=pb_: e.matmul(
                        pb_.t[:, :], lhsT=ident_b.t[:, :], rhs=vs.t[:, half * 512:(half + 1) * 512],
                        start=(s_ == 0), stop=(s_ == 127)),
                         r=[ident_b, vs], **({'w': [pb_]} if s_ == 0 else {'pw': [pb_]}))

            def final():
                for half, pb_ in ((0, pa), (1, pb)):
                    k.op('dve', lambda e, half=half, pb_=pb_: e.tensor_tensor(
                        out=h.t[:, half * 512:(half + 1) * 512], in0=h.t[:, half * 512:(half + 1) * 512],
                        in1=pb_.t[:, :], op=ALU.add), r=[pb_, h], pw=[h])
                if fin is not None:
                    fin()

            B = pipe.n
            for s_ in range(128):
                pipe.add(B + s_, lambda s_=s_: G(s_))
                pipe.add(B + s_, lambda s_=s_: Dt(s_))
                pipe.add(B + s_ + 1, lambda s_=s_: P(s_))
                pipe.add(B + s_ + 2, lambda s_=s_: R(s_))
                pipe.add(B + s_ + 3, lambda s_=s_: E(s_))
                pipe.add(B + s_ + 4, lambda s_=s_: X(s_))
                pipe.add(B + s_ + 5, lambda s_=s_: W(s_))
                pipe.add(B + s_ + 6, lambda s_=s_: M(s_))
            pipe.add(B + 127 + 6, final)
            for s_ in range(128):
                pipe.run(B + s_)
                if rec and s_ >= 8:
                    nd_left = sum(1 for r_ in rec if r_[0] == 'op' and r_[1][0] == 'dve')
                    left = max(1, 108 - s_)
                    k.replay(rec, max(8, (len(rec) + left - 1) // left + 4), ndve=min(4, max(1, (nd_left + left - 1) // left)))
                if rec and s_ == 120:
                    k.replay(rec, len(rec))
            if rec:
                k.replay(rec, len(rec))
            pipe.n = B + 128

        with ExitStack() as p1:
            gains = k.sb(p1, "gains", [128, 3, D], F32)
            for gi in range(3):
                k.dma('sp', 'gn%d' % gi, lambda e, gi=gi: e.dma_start(
                    out=gains.t[:, gi, :], in_=gains_d[gi:gi + 1, :].partition_broadcast(128)), pw=[gains])
            poolw = k.sb(p1, "poolw", [128, 8, 256], BF16)
            for g in range(4):
                for cc in range(2):
                    k.dma('pool', 'pw%d' % cc, lambda e, g=g, cc=cc: e.dma_start(
                        out=poolw.t[:, g * 2 + cc, :], in_=poolw_d[g, cc * 128:(cc + 1) * 128, :]), pw=[poolw])
            bands = k.sb(p1, "bands", [128, 12, 128], BF16)
            k.dma('pool', 'bands', lambda e: e.dma_start(out=bands.t[:, :, :], in_=bands_d[:, :, :]), w=[bands])
            st = peer_alloc(p1)
            peer_load_weights(st, 0)
            xt = [k.sb(p1, "xt%d" % i, [128, D], F32) for i in range(3)]
            hn = [k.sb(p1, "hn%d" % i, [128, D], BF16) for i in range(2)]
            pooledT = k.sb(p1, "pooledT", [128, D], BF16)
            ms1 = k.sb(p1, "ms1", [128, 1], F32)
            rstd1 = k.sb(p1, "rstd1", [128, 1], F32)
            mixs = k.sb(p1, "mixs", [128, D], F32)

            def load1(i):
                x_ = xt[i % 3]
                k.dma('sp', 'x%d' % (i % 3), lambda e: e.dma_start(out=x_.t[:, :], in_=xpad[i * 128:(i + 1) * 128, :]),
                      w=[x_])

            def front1(i):
                par = i % 2
                x_ = xt[i % 3]
                rstd_pow(x_, D, ms1, rstd1, st.junk)
                k.op('dve', lambda e: e.scalar_tensor_tensor(out=hn[par].t[:, :], in0=x_.t[:, :], scalar=rstd1.t[:, :],
                                                             in1=gains.t[:, 0, :], op0=ALU.mult, op1=ALU.mult),
                     r=[x_, rstd1, gains], w=[hn[par]])
                for bnk in range(2):
                    pp = ps[(0, 5)[bnk]]
                    for cc in range(4):
                        c = bnk * 4 + cc
                        g = c // 2
                        bcur = (8 + g) if i == 0 else g
                        k.op('pe', lambda e, c=c, cc=cc, pp=pp, bcur=bcur: e.matmul(
                            pp.t[:, cc * 128:(cc + 1) * 128], lhsT=hn[par].t[:, c * 128:(c + 1) * 128],
                            rhs=bands.t[:, bcur, :], start=True, stop=(i == 0)),
                             r=[hn[par], bands], **({'w': [pp]} if cc == 0 else {'pw': [pp]}))
                        if i > 0:
                            k.op('pe', lambda e, c=c, cc=cc, pp=pp, g=g: e.matmul(
                                pp.t[:, cc * 128:(cc + 1) * 128], lhsT=hn[1 - par].t[:, c * 128:(c + 1) * 128],
                                rhs=bands.t[:, 4 + g, :], start=False, stop=True),
                                 r=[hn[1 - par], bands], pw=[pp])
                    k.op('act', lambda e, bnk=bnk, pp=pp: e.activation(
                        out=pooledT.t[:, bnk * 512:(bnk + 1) * 512], in_=pp.t[:, :], func=AF.Copy),
                         r=[pp], **({'w': [pooledT]} if bnk == 0 else {'pw': [pooledT]}))
                for bnk in range(2):
                    pp = ps[(0, 5)[bnk]]
                    for gg in range(2):
                        g = bnk * 2 + gg
                        for cc in range(2):
                            c = g * 2 + cc
                            k.op('pe', lambda e, c=c, gg=gg, cc=cc, pp=pp: e.matmul(
                                pp.t[:, gg * 256:(gg + 1) * 256], lhsT=pooledT.t[:, c * 128:(c + 1) * 128],
                                rhs=poolw.t[:, c, :], start=(cc == 0), stop=(cc == 1)),
                                 r=[pooledT, poolw], **({'w': [pp]} if (gg == 0 and cc == 0) else {'pw': [pp]}))
                    sl = slice(bnk * 512, (bnk + 1) * 512)
                    k.op('dve', lambda e, pp=pp, sl=sl: e.tensor_tensor(
                        out=mixs.t[:, sl], in0=pp.t[:, :], in1=gains.t[:, 2, sl], op=ALU.mult),
                         r=[pp, gains], **({'w': [mixs]} if bnk == 0 else {'pw': [mixs]}))
                k.op('dve', lambda e: e.tensor_tensor(out=x_.t[:, :], in0=x_.t[:, :], in1=mixs.t[:, :], op=ALU.add),
                     r=[mixs], w=[x_])
                peer_prep(st, x_, gains.t[:, 1, :], gains, par)

            pipe1 = Pipe()

            def back1(i, rec=None):
                par = i % 2
                x_ = xt[i % 3]
                peer_gather(pipe1, st, 0, x_, par, (ps[6], ps[7]), rec,
                            fin=lambda: k.dma('sp', 'h2st%d' % par, lambda e: e.dma_start(
                                out=H2[i * 128:(i + 1) * 128, :], in_=x_.t[:, :]), r=[x_]))

            load1(0)
            if NT > 1:
                load1(1)
            front1(0)
            for i in range(NT):
                k.rec = rec = []
                if i + 2 < NT:
                    load1(i + 2)
                if i + 1 < NT:
                    front1(i + 1)
                k.rec = None
                back1(i, rec)
            pipe1.flush()
            k.barrier()

        with ExitStack() as p2:
            gains2 = k.sb(p2, "gains2", [128, 2, D], F32)
            for gi, src in ((0, 3), (1, 4)):
                k.dma('sp', 'gn%d' % gi, lambda e, gi=gi, src=src: e.dma_start(
                    out=gains2.t[:, gi, :], in_=gains_d[src:src + 1, :].partition_broadcast(128)), pw=[gains2])
            hnm = k.sb(p2, "hnm", [128, 2, 64], F32)
            for gi in range(2):
                k.dma('sp', 'hn%d' % gi, lambda e, gi=gi: e.dma_start(
                    out=hnm.t[:, gi, :], in_=hnorm_d[gi:gi + 1, :].partition_broadcast(128)), pw=[hnm])
            k.op('dve', lambda e: e.tensor_scalar(out=hnm.t[:, 1, :], in0=hnm.t[:, 1, :], scalar1=0.125, scalar2=None,
                                                  op0=ALU.mult), r=[hnm], w=[hnm])
            wkv = k.sb(p2, "wkv", [128, 8, 2048], BF16)
            wqa = k.sb(p2, "wqa", [128, 8, D], BF16)
            for c in range(8):
                k.dma('pool', 'wq%d' % (c % 2), lambda e, c=c: e.dma_start(
                    out=wkv.t[:, c, :], in_=wkv_d[c * 128:(c + 1) * 128, :]), pw=[wkv])
                k.dma('pool', 'pw%d' % (c % 2), lambda e, c=c: e.dma_start(
                    out=wqa.t[:, c, :], in_=wqa_d[c * 128:(c + 1) * 128, :]), pw=[wqa])
            ht = [k.sb(p2, "ht%d" % i, [128, D], F32) for i in range(2)]
            junk2 = k.sb(p2, "junk2", [128, D], F32)
            ms2 = k.sb(p2, "ms2", [128, 1], F32)
            rstd2 = k.sb(p2, "rstd2", [128, 1], F32)
            nb_ = [k.sb(p2, "nb%d" % i, [128, D], BF16) for i in range(2)]
            nT = [k.sb(p2, "nT%d" % i, [128, D], BF16) for i in range(2)]
            sq = k.sb(p2, "sq", [128, D], F32)
            ss = k.sb(p2, "ss", [128, 16], F32)
            tmpn = k.sb(p2, "tmpn", [128, D], F32)
            knb = k.sb(p2, "knb", [128, D], BF16)
            kT = [k.sb(p2, "kT%d" % i, [128, D], BF16) for i in range(2)]
            vb = [k.sb(p2, "vb%d" % i, [128, D], BF16) for i in range(2)]

            def headnorm_T(src_banks, gi, dstT, dram, i, tag):
                for half, pb_ in enumerate(src_banks):
                    sl = slice(half * 512, (half + 1) * 512)
                    k.op('act', lambda e, pb_=pb_, sl=sl: e.activation(out=sq.t[:, sl], in_=pb_.t[:, :], func=AF.Square),
                         r=[pb_], **({'w': [sq]} if half == 0 else {'pw': [sq]}))
                k.op('dve', lambda e: e.tensor_reduce(out=ss.t[:, :], in_=sq.t[:, :].rearrange("p (h d) -> p h d", d=64),
                                                      axis=AX.X, op=ALU.add), r=[sq], w=[ss])
                k.op('act', lambda e: e.activation(out=ss.t[:, :], in_=ss.t[:, :], func=AF.Ln, bias=epsb.t[:, :],
                                                   scale=1.0 / 64), r=[ss, epsb], w=[ss])
                k.op('act', lambda e: e.activation(out=ss.t[:, :], in_=ss.t[:, :], func=AF.Exp, scale=-0.5),
                     r=[ss], w=[ss])
                for half, pb_ in enumerate(src_banks):
                    sl = slice(half * 512, (half + 1) * 512)
                    k.op('dve', lambda e, pb_=pb_, sl=sl, half=half: e.tensor_tensor(
                        out=tmpn.t[:, sl].rearrange("p (h d) -> p h d", d=64),
                        in0=pb_.t[:, :].rearrange("p (h d) -> p h d", d=64),
                        in1=ss.t[:, half * 8:(half + 1) * 8].unsqueeze(2).to_broadcast([128, 8, 64]), op=ALU.mult),
                         r=[pb_, ss], **({'w': [tmpn]} if half == 0 else {'pw': [tmpn]}))
                k.op('dve', lambda e: e.tensor_tensor(
                    out=knb.t[:, :].rearrange("p (h d) -> p h d", d=64),
                    in0=tmpn.t[:, :].rearrange("p (h d) -> p h d", d=64),
                    in1=hnm.t[:, gi, :].unsqueeze(1).to_broadcast([128, 16, 64]), op=ALU.mult),
                     r=[tmpn, hnm], w=[knb])
                pT = src_banks[0]
                pTb = pT.t[:, :].bitcast(BF16)
                for c in range(8):
                    k.op('pe', lambda e, c=c: e.transpose(out=pTb[:, c * 128:(c + 1) * 128],
                                                          in_=knb.t[:, c * 128:(c + 1) * 128], identity=ident_b.t[:, :]),
                         r=[knb, ident_b], **({'w': [pT]} if c == 0 else {'pw': [pT]}))
                k.op('act', lambda e: e.activation(out=dstT.t[:, :], in_=pTb, func=AF.Copy), r=[pT], w=[dstT])
                for hh in range(2):
                    k.dma('sp', '%s%d_%d' % (tag, i % 2, hh), lambda e, hh=hh: e.dma_start(
                        out=dram.rearrange("(pr two) d l -> two d pr l", two=2)[hh, :, :, i * 128:(i + 1) * 128],
                        in_=dstT.t[hh * 64:(hh + 1) * 64, :].rearrange("p (pr t) -> p pr t", t=128)),
                          r=[dstT])

            for i in range(NT):
                par = i % 2
                h_ = ht[par]
                k.dma('sp', 'x%d' % par, lambda e: e.dma_start(out=h_.t[:, :], in_=H2[i * 128:(i + 1) * 128, :]), w=[h_])
                rstd_of(h_, D, ms2, rstd2, junk2)
                for gi in range(2):
                    k.op('dve', lambda e, gi=gi: e.scalar_tensor_tensor(
                        out=nb_[gi].t[:, :], in0=h_.t[:, :], scalar=rstd2.t[:, :], in1=gains2.t[:, gi, :],
                        op0=ALU.mult, op1=ALU.mult), r=[h_, rstd2, gains2], w=[nb_[gi]])
                    pT = ps[gi]
                    pTb = pT.t[:, :].bitcast(BF16)
                    for c in range(8):
                        k.op('pe', lambda e, c=c, gi=gi, pTb=pTb: e.transpose(
                            out=pTb[:, c * 128:(c + 1) * 128], in_=nb_[gi].t[:, c * 128:(c + 1) * 128],
                            identity=ident_b.t[:, :]),
                             r=[nb_[gi], ident_b], **({'w': [pT]} if c == 0 else {'pw': [pT]}))
                    k.op('act', lambda e, gi=gi, pTb=pTb: e.activation(out=nT[gi].t[:, :], in_=pTb, func=AF.Copy),
                         r=[pT], w=[nT[gi]])
                for blk in range(4):
                    pb_ = ps[2 + blk]
                    for c in range(8):
                        k.op('pe', lambda e, c=c, blk=blk, pb_=pb_: e.matmul(
                            pb_.t[:, :], lhsT=nT[0].t[:, c * 128:(c + 1) * 128],
                            rhs=wkv.t[:, c, blk * 512:(blk + 1) * 512], start=(c == 0), stop=(c == 7)),
                             r=[nT[0], wkv], **({'w': [pb_]} if c == 0 else {'pw': [pb_]}))
                for blk in range(2):
                    pb_ = ps[6 + blk]
                    for c in range(8):
                        k.op('pe', lambda e, c=c, blk=blk, pb_=pb_: e.matmul(
                            pb_.t[:, :], lhsT=nT[1].t[:, c * 128:(c + 1) * 128],
                            rhs=wqa.t[:, c, blk * 512:(blk + 1) * 512], start=(c == 0), stop=(c == 7)),
                             r=[nT[1], wqa], **({'w': [pb_]} if c == 0 else {'pw': [pb_]}))
                v_ = vb[par]
                for half in range(2):
                    k.op('act', lambda e, half=half: e.activation(
                        out=v_.t[:, half * 512:(half + 1) * 512], in_=ps[4 + half].t[:, :], func=AF.Copy),
                         r=[ps[4 + half]], **({'w': [v_]} if half == 0 else {'pw': [v_]}))
                k.dma('sp', 'vst%d' % par, lambda e: e.dma_start(out=VV[i * 128:(i + 1) * 128, :], in_=v_.t[:, :]), r=[v_])
                headnorm_T((ps[2], ps[3]), 0, kT[0], KT, i, 'kst')
                headnorm_T((ps[6], ps[7]), 1, kT[1], QT, i, 'qst')
            k.barrier()

        with ExitStack() as p3:
            kth = [k.sb(p3, "kth%d" % i, [64, Lp], BF16) for i in range(2)]
            qth = [k.sb(p3, "qth%d" % i, [64, Lp], BF16) for i in range(2)]
            vh = [k.sb(p3, "vh%d" % i, [128, NT, 64], BF16) for i in range(2)]
            e_sb = [k.sb(p3, "e_sb%d" % i, [128, 512], F32) for i in range(2)]
            sp_sb = [k.sb(p3, "sp_sb%d" % i, [128, 512], BF16) for i in range(3)]
            wg_sb = [k.sb(p3, "wg_sb%d" % i, [128, 512], BF16) for i in range(3)]
            S = [k.sb(p3, "S%d" % i, [128, 512], BF16) for i in range(2)]
            o_sb = [k.sb(p3, "o_sb%d" % i, [64, 512], BF16) for i in range(2)]
            convert_tables(1)
            one = k.sb(p3, "onec", [128, 1], F32)
            k.op('dve', lambda e: e.memset(one.t[:, :], 1.0), w=[one])
            NA = 5
            tasks = []

            def load_head(head):
                hp = head % 2
                k.dma('sp', 'kth%d' % hp, lambda e: e.dma_start(out=kth[hp].t[:, :], in_=KT[head, :, :]), w=[kth[hp]])
                k.dma('sp', 'qth%d' % hp, lambda e: e.dma_start(out=qth[hp].t[:, :], in_=QT[head, :, :]), w=[qth[hp]])
                k.dma('sp', 'vh%d' % hp, lambda e: e.dma_start(
                    out=vh[hp].t[:, :, :],
                    in_=VV.rearrange("(j p) d -> p j d", p=128)[:, :, head * 64:(head + 1) * 64]), w=[vh[hp]])

            def make_task(t, head, qa, qb, j, n, g):
                hp = head % 2
                ncols = (qb - qa + 1) * 128
                cs = max(j - qa, 0) * 128
                A = ps[t % NA]
                eb_ = e_sb[t % 2]
                spb = sp_sb[t % 3]
                wgb = wg_sb[t % 3]
                O = ps[NA + g % 2]
                osb = o_sb[g % 2]
                diag = j >= qa
                first = (n == 0)
                last = (j == 0)
                Sold = S[(n - 1) % 2]
                Snew = S[n % 2]

                def s1():
                    if first and qa == 1 and head + 1 < 16:
                        load_head(head + 1)
                    if first:
                        k.op('dve', lambda e: e.memset(O.t[0:64, :], 0.0), w=[O])
                    k.op('pe', lambda e: e.matmul(
                        A.t[:, cs:ncols], lhsT=kth[hp].t[:, j * 128:(j + 1) * 128],
                        rhs=qth[hp].t[:, qa * 128 + cs:qa * 128 + ncols], start=True, stop=False),
                         r=[kth[hp], qth[hp]], w=[A])

                def s2():
                    k.op('act', lambda e: e.activation(
                        out=eb_.t[:, cs:ncols], in_=A.t[:, cs:ncols], func=AF.Exp), r=[A], w=[eb_])
                    k.op('act', lambda e: e.activation(
                        out=spb.t[:, cs:ncols], in_=eb_.t[:, cs:ncols], func=AF.Ln, bias=one.t[:, :], scale=1.0),
                         r=[eb_, one], w=[spb])
                    if diag:
                        k.op('dve', lambda e: e.tensor_tensor(
                            out=spb.t[:, cs:cs + 128], in0=spb.t[:, cs:cs + 128], in1=trim.t[:, :], op=ALU.mult),
                             r=[spb, trim], w=[spb])
                    if j == 0:
                        k.op('dve', lambda e: e.tensor_scalar(
                            out=spb.t[:, cs:ncols], in0=spb.t[:, cs:ncols], scalar1=rowm.t[:, :], scalar2=None,
                            op0=ALU.mult), r=[spb, rowm], w=[spb])

                def s3():
                    k.op('pe', lambda e: e.matmul(
                        A.t[:, cs:ncols], lhsT=negtri.t[:, :], rhs=spb.t[:, cs:ncols], start=False, stop=first),
                         r=[negtri, spb], pw=[A])
                    if not first:
                        k.op('pe', lambda e: e.matmul(
                            A.t[:, cs:ncols], lhsT=negones.t[:, :], rhs=Sold.t[:, cs:ncols], start=False, stop=True),
                             r=[negones, Sold], pw=[A])
                    if j > 0:
                        if first:
                            k.op('dve', lambda e: e.memset(Snew.t[:, :], 0.0), w=[Snew])
                            k.op('dve', lambda e: e.tensor_copy(
                                out=Snew.t[:, cs:ncols], in_=spb.t[:, cs:ncols]), r=[spb], w=[Snew])
                        elif cs > 0:
                            k.op('dve', lambda e: e.memset(Snew.t[:, 0:cs], 0.0), w=[Snew])
                            k.op('dve', lambda e: e.tensor_tensor(
                                out=Snew.t[:, cs:ncols], in0=Sold.t[:, cs:ncols], in1=spb.t[:, cs:ncols],
                                op=ALU.add), r=[Sold, spb], pw=[Snew])
                        else:
                            k.op('dve', lambda e: e.tensor_tensor(
                                out=Snew.t[:, 0:ncols], in0=Sold.t[:, 0:ncols], in1=spb.t[:, 0:ncols],
                                op=ALU.add), r=[Sold, spb], w=[Snew])

                def s4():
                    k.op('act', lambda e: e.activation(
                        out=wgb.t[:, cs:ncols], in_=A.t[:, cs:ncols], func=AF.Exp), r=[A], w=[wgb])
                    if diag:
                        k.op('dve', lambda e: e.tensor_tensor(
                            out=wgb.t[:, cs:cs + 128], in0=wgb.t[:, cs:cs + 128], in1=trim.t[:, :], op=ALU.mult),
                             r=[wgb, trim], w=[wgb])
                    if j == 0:
                        k.op('dve', lambda e: e.tensor_scalar(
                            out=wgb.t[:, cs:ncols], in0=wgb.t[:, cs:ncols], scalar1=rowm.t[:, :], scalar2=None,
                            op0=ALU.mult), r=[wgb, rowm], w=[wgb])

                def s5():
                    k.op('pe', lambda e: e.matmul(
                        O.t[0:64, cs:ncols], lhsT=vh[hp].t[:, j, :], rhs=wgb.t[:, cs:ncols],
                        start=False, stop=last, skip_group_check=True),
                         r=[vh[hp], wgb], pw=[O])
                    if last:
                        k.op('act', lambda e: e.activation(
                            out=osb.t[:, 0:ncols], in_=O.t[0:64, 0:ncols], func=AF.Copy), r=[O], w=[osb])
                        k.dma('sp', 'ost' + osb.name, lambda e: e.dma_start(
                            out=AT[head * 64:(head + 1) * 64, qa * 128:qa * 128 + ncols], in_=osb.t[:, 0:ncols]),
                              r=[osb])
                return (s1, s2, s3, s4, s5)

            t = 0
            g = 0
            for head in range(16):
                for qa in range(1, NT, 4):
                    qb = min(qa + 3, NT - 1)
                    for n, j in enumerate(range(qb, -1, -1)):
                        tasks.append(make_task(t, head, qa, qb, j, n, g))
                        t += 1
                    g += 1
            load_head(0)
            NTK = len(tasks)
            for step in range(NTK + 4):
                for st_i in (4, 3, 2, 1, 0):
                    ti = step - st_i
                    if 0 <= ti < NTK:
                        tasks[ti][st_i]()
            k.barrier()

        with ExitStack() as p4:
            gains4 = k.sb(p4, "gains4", [128, D], F32)
            k.dma('sp', 'gn0', lambda e: e.dma_start(out=gains4.t[:, :], in_=gains_d[5:6, :].partition_broadcast(128)),
                  w=[gains4])
            wo = k.sb(p4, "wo", [128, 8, D], BF16)
            for c in range(8):
                k.dma('pool', 'pw%d' % (c % 2), lambda e, c=c: e.dma_start(
                    out=wo.t[:, c, :], in_=wo_d[c * 128:(c + 1) * 128, :]), pw=[wo])
            st = peer_alloc(p4)
            peer_load_weights(st, 1)
            h4 = [k.sb(p4, "h4_%d" % i, [128, D], F32) for i in range(3)]
            att = [k.sb(p4, "att%d" % i, [128, 8, 128], BF16) for i in range(3)]

            def load4(i):
                h_, a_ = h4[i % 3], att[i % 3]
                k.dma('sp', 'x%d' % (i % 3), lambda e: e.dma_start(out=h_.t[:, :], in_=H2[i * 128:(i + 1) * 128, :]), w=[h_])
                k.dma('sp', 'att%d' % (i % 3), lambda e: e.dma_start(
                    out=a_.t[:, :, :],
                    in_=AT.rearrange("(c p) l -> p c l", p=128)[:, :, i * 128:(i + 1) * 128]), w=[a_])

            def front4(i):
                par = i % 2
                h_ = h4[i % 3]
                for half in range(2):
                    pb_ = ps[(0, 5)[half]]
                    for c in range(8):
                        k.op('pe', lambda e, c=c, half=half, pb_=pb_: e.matmul(
                            pb_.t[:, :], lhsT=att[i % 3].t[:, c, :], rhs=wo.t[:, c, half * 512:(half + 1) * 512],
                            start=(c == 0), stop=(c == 7)),
                             r=[att[i % 3], wo], **({'w': [pb_]} if c == 0 else {'pw': [pb_]}))
                    sl = slice(half * 512, (half + 1) * 512)
                    k.op('dve', lambda e, pb_=pb_, sl=sl: e.tensor_tensor(
                        out=h_.t[:, sl], in0=h_.t[:, sl], in1=pb_.t[:, :], op=ALU.add), r=[pb_, h_], pw=[h_])
                peer_prep(st, h_, gains4.t[:, :], gains4, par)

            pipe4 = Pipe()

            def back4(i, rec=None):
                par = i % 2
                h_ = h4[i % 3]
                peer_gather(pipe4, st, 1, h_, par, (ps[6], ps[7]), rec,
                            fin=lambda: k.dma('sp', 'h2st%d' % par, lambda e: e.dma_start(
                                out=out_d[(i - 1) * 128:i * 128, :], in_=h_.t[:, :]), r=[h_]))

            load4(1)
            if NT > 2:
                load4(2)
            front4(1)
            for i in range(1, NT):
                k.rec = rec = []
                if i + 2 < NT:
                    load4(i + 2)
                if i + 1 < NT:
                    front4(i + 1)
                k.rec = None
                back4(i, rec)
            pipe4.flush()
            k.barrier()
        print("build: ninst=%d" % k.ninst, {e: k.cnt[e] for e in COMPUTE})
    return nc


def _consts():
    ident = np.eye(128, dtype=np.float32)
    kk = np.arange(128)
    trim = (kk[:, None] < kk[None, :]).astype(np.float32)
    negtri = -(kk[:, None] >= kk[None, :]).astype(np.float32)
    negones = -np.ones((128, 128), np.float32)
    cst = np.stack([ident, trim, negtri, negones], axis=1)
    rowm = (kk >= 112).astype(np.float32)[:, None]
    iota = np.tile(np.arange(16, dtype=np.float32), 128)[None, :].repeat(128, axis=0)
    bands = np.zeros((128, 12, 128), np.float32)
    for g, w in enumerate((2, 4, 8, 16)):
        for t in range(128):
            for tp in range(t - w + 1, t + 1):
                if tp >= 0:
                    bands[tp, g, t] += 1.0 / w
                else:
                    bands[128 + tp, 4 + g, t] += 1.0 / w
            bands[t, g, t] -= 1.0
            l = t - 112
            c = float(min(max(l, 0) + 1, w))
            for tp in range(t - w + 1, t + 1):
                if tp >= 0:
                    bands[tp, 8 + g, t] += 1.0 / c
            bands[t, 8 + g, t] -= 1.0
    return cst, rowm, iota, bands


def _in_maps(inputs, NT, cores):
    x = np.asarray(inputs["x"], np.float32)
    meta = np.asarray(inputs["meta_tokens"], np.float32)
    Lp = NT * 128
    cst, rowm, iota, bands = _consts()
    gains = np.stack([inputs["norm_mix"][0], inputs["norm_ffn"][0], inputs["pool_scale"][0], inputs["kv_norm"],
                      inputs["norm_mix"][1], inputs["norm_ffn"][1]]).astype(np.float32)
    hnorm = np.stack([inputs["k_norm"], inputs["q_norm"][0]]).astype(np.float32)
    keysT = np.ascontiguousarray(np.transpose(np.asarray(inputs["peer_keys"], np.float32), (0, 4, 1, 2, 3))
                                 ).reshape(2, 128, 2048)
    shared = {
        "gains": np.ascontiguousarray(gains), "hnorm": np.ascontiguousarray(hnorm),
        "poolw": np.ascontiguousarray(inputs["pool_w"][0], dtype=np.float32), "bands": bands,
        "wq": np.ascontiguousarray(inputs["peer_wq"], dtype=np.float32), "keysT": keysT,
        "wkv": np.ascontiguousarray(inputs["w_kv"], dtype=np.float32),
        "wqa": np.ascontiguousarray(inputs["w_q"][0], dtype=np.float32),
        "wo": np.ascontiguousarray(inputs["w_o"][0], dtype=np.float32),
        "pu0": np.ascontiguousarray(inputs["peer_u"][0], dtype=np.float32),
        "pu1": np.ascontiguousarray(inputs["peer_u"][1], dtype=np.float32),
        "pv0": np.ascontiguousarray(inputs["peer_v"][0], dtype=np.float32),
        "pv1": np.ascontiguousarray(inputs["peer_v"][1], dtype=np.float32),
        "cst": cst, "rowm": rowm, "iota": iota,
    }
    maps = []
    for b in cores:
        xp = np.zeros((Lp, D), np.float32)
        xp[112:128] = meta
        xp[128:] = x[b, :Lp - 128]
        m = dict(shared)
        m["xpad"] = xp
        maps.append(m)
    return maps


def run(inputs, NT, cores, dbg=False):
    nc = build(NT, dbg=dbg)
    maps = _in_maps(inputs, NT, cores)
    res = run_bass_kernel_spmd(nc, maps, core_ids=list(range(len(cores))))
    return res


def kernel(**inputs):
    NT = 65
    res = run(inputs, NT, list(range(8)))
    return np.stack([np.asarray(r["out"], np.float32) for r in res.results], axis=0)
```

```python
import numpy as np
from contextlib import ExitStack
import concourse.bass as bass
import concourse.mybir as mybir
from concourse.alu_op_type import AluOpType as ALU
from concourse.bass_utils import run_bass_kernel_spmd

F32 = mybir.dt.float32
BF16 = mybir.dt.bfloat16
U32 = mybir.dt.uint32
I32 = mybir.dt.int32
AF = mybir.ActivationFunctionType
AX = mybir.AxisListType

D = 1024
EPS = 1e-6
NEG = -1.0e30
NB = 10
COMPUTE = ('pe', 'act', 'dve', 'pool')


class Buf:
    __slots__ = ('t', 'w', 'fw', 'r', 'name')

    def __init__(self, t, name=''):
        self.t = t
        self.w = {}
        self.fw = {}
        self.r = {}
        self.name = name


class K:
    def __init__(self, nc, es):
        self.nc = nc
        self.es = es
        self.engs = {'pe': nc.tensor, 'act': nc.scalar, 'dve': nc.vector,
                     'pool': nc.gpsimd, 'sp': nc.sync}
        self.semh = {}
        self.cnt = {}
        for e in COMPUTE:
            self.semh[e] = es.enter_context(nc.semaphore('sem_' + e))
            self.cnt[e] = 0
        self.seen = {e: {} for e in self.engs}
        self.ninst = 0

    def sb(self, es, name, shape, dt):
        self.nalloc = getattr(self, 'nalloc', 0) + 1
        return Buf(es.enter_context(self.nc.sbuf_tensor("s%d_%s" % (self.nalloc, name), list(shape), dt)), name)

    def _dsem(self, key):
        if key not in self.semh:
            self.semh[key] = self.es.enter_context(self.nc.semaphore('d_' + key))
            self.cnt[key] = 0
        return self.semh[key]

    def _wait(self, e, deps):
        seen = self.seen[e]
        for key, val in deps.items():
            if val <= 0 or seen.get(key, 0) >= val:
                continue
            self.engs[e].wait_ge(self.semh[key], val)
            seen[key] = val
            self.ninst += 1

    @staticmethod
    def _merge(d, s):
        for k_, v in s.items():
            if d.get(k_, 0) < v:
                d[k_] = v

    def _deps(self, r, w, pw):
        deps = {}
        for b in r:
            self._merge(deps, b.w)
        for b in w:
            self._merge(deps, b.w)
            self._merge(deps, b.r)
        for b in pw:
            self._merge(deps, b.fw)
            self._merge(deps, b.r)
        return deps

    def _mark(self, key, val, r, w, pw):
        for b in r:
            if b.r.get(key, 0) < val:
                b.r[key] = val
        for b in w:
            b.w = {key: val}
            b.fw = {key: val}
            b.r = {}
        for b in pw:
            if b.w.get(key, 0) < val:
                b.w[key] = val

    rec = None

    def replay(self, rec, n, ndve=None):
        saved, self.rec = self.rec, None
        nd = 0
        for _ in range(min(n, len(rec))):
            kind, args, kw = rec[0]
            if ndve is not None and kind == 'op' and args[0] == 'dve':
                if nd >= ndve:
                    break
                nd += 1
            rec.pop(0)
            (self.op if kind == 'op' else self.dma)(*args, **kw)
        self.rec = saved

    def op(self, e, fn, r=(), w=(), pw=()):
        if self.rec is not None:
            self.rec.append(('op', (e, fn), dict(r=r, w=w, pw=pw)))
            return
        self._wait(e, self._deps(r, w, pw))
        inst = fn(self.engs[e])
        self.cnt[e] += 1
        inst.then_inc(self.semh[e], 1)
        self.ninst += 1
        self._mark(e, self.cnt[e], r, w, pw)

    def dma(self, q, key, fn, r=(), w=(), pw=()):
        if self.rec is not None:
            self.rec.append(('dma', (q, key, fn), dict(r=r, w=w, pw=pw)))
            return
        self._dsem(key)
        deps = self._deps(r, w, pw)
        if self.cnt[key] > 0:
            deps[key] = max(deps.get(key, 0), self.cnt[key])
        self._wait(q, deps)
        inst = fn(self.engs[q])
        self.cnt[key] += 16
        inst.then_inc(self.semh[key], 16)
        self.ninst += 1
        self._mark(key, self.cnt[key], r, w, pw)

    def barrier(self):
        alld = {key: v for key, v in self.cnt.items() if v > 0}
        for e in self.engs:
            self._wait(e, alld)


def bcast_rows(ap2d_row, nparts):
    return ap2d_row.partition_broadcast(nparts)


def build(NT, dbg=False):
    nc = bass.Bass("TRN2", target_bir_lowering=False)
    Lp = NT * 128
    NQ = NT - 1
    din = lambda name, shape, dt=F32: nc.dram_tensor(name, list(shape), dt, kind="ExternalInput").ap()
    xpad = din("xpad", [Lp, D])
    gains_d = din("gains", [6, D])
    hnorm_d = din("hnorm", [2, 64])
    poolw_d = din("poolw", [4, 256, 256])
    bands_d = din("bands", [128, 12, 128])
    wq_d = din("wq", [2, D, 2048])
    keysT_d = din("keysT", [2, 128, 2048])
    wkv_d = din("wkv", [D, 2048])
    wqa_d = din("wqa", [D, D])
    wo_d = din("wo", [D, D])
    pu_d = [din("pu%d" % l, [16384, D]) for l in range(2)]
    pv_d = [din("pv%d" % l, [16384, D]) for l in range(2)]
    cst_d = din("cst", [128, 4, 128])
    rowm_d = din("rowm", [128, 1])
    iota_d = din("iota", [128, 2048])
    out_d = nc.dram_tensor("out", [NQ * 128, D], F32, kind="ExternalOutput").ap()
    skind = "ExternalOutput" if dbg else "Internal"
    H2 = nc.dram_tensor("H2", [Lp, D], F32, kind=skind).ap()
    KT = nc.dram_tensor("KT", [16, 64, Lp], BF16, kind=skind).ap()
    QT = nc.dram_tensor("QT", [16, 64, Lp], BF16, kind=skind).ap()
    VV = nc.dram_tensor("VV", [Lp, D], BF16, kind=skind).ap()
    AT = nc.dram_tensor("AT", [D, Lp], BF16, kind=skind).ap()
    UVb = [nc.dram_tensor("UVb%d" % l, [16384, 2 * D], BF16, kind="Internal").ap() for l in range(2)]

    with ExitStack() as es:
        k = K(nc, es)
        ps = [Buf(es.enter_context(nc.psum_tensor("ps%d" % i, [128, 512], F32)), "ps%d" % i)
              for i in range(8)]
        ident_b = k.sb(es, "ident_b", [128, 128], BF16)
        ident_f = k.sb(es, "ident_f", [128, 128], F32)
        trim = k.sb(es, "trim", [128, 128], BF16)
        negtri = k.sb(es, "negtri", [128, 128], BF16)
        negones = k.sb(es, "negones", [128, 128], BF16)
        rowm = k.sb(es, "rowm", [128, 1], F32)
        epsb = k.sb(es, "epsb", [128, 1], F32)
        k.dma('pool', 'c0', lambda e: e.dma_start(out=ident_b.t[:, :], in_=cst_d[:, 0, :]), w=[ident_b])
        k.dma('sp', 'c1', lambda e: e.dma_start(out=ident_f.t[:, :], in_=cst_d[:, 0, :]), w=[ident_f])
        k.dma('pool', 'c2', lambda e: e.dma_start(out=trim.t[:, :], in_=cst_d[:, 1, :]), w=[trim])
        k.dma('pool', 'c3', lambda e: e.dma_start(out=negtri.t[:, :], in_=cst_d[:, 2, :]), w=[negtri])
        k.dma('pool', 'c4', lambda e: e.dma_start(out=negones.t[:, :], in_=cst_d[:, 3, :]), w=[negones])
        k.dma('sp', 'c5', lambda e: e.dma_start(out=rowm.t[:, :], in_=rowm_d[:, :]), w=[rowm])
        k.op('dve', lambda e: e.memset(epsb.t[:, :], EPS), w=[epsb])

        uvb = [Buf(None, "uvb%d" % l) for l in range(2)]

        def convert_tables(l):
            for which, src in ((0, pu_d[l]), (1, pv_d[l])):
                for rr in range(4):
                    k.dma('pool', 'cv%d' % ((which * 4 + rr) % 4), lambda e, l=l, which=which, src=src, rr=rr: e.dma_start(
                        out=UVb[l][rr * 4096:(rr + 1) * 4096, which * D:(which + 1) * D],
                        in_=src[rr * 4096:(rr + 1) * 4096, :]), pw=[uvb[l]])

        convert_tables(0)

        def rstd_of(src, n_feat, ms, rstd, junk, src_ap=None):
            sap = src.t[:, :] if src_ap is None else src_ap
            k.op('act', lambda e: e.activation(out=junk.t[:, :], in_=sap, func=AF.Square,
                                               scale=float(n_feat) ** -0.5, accum_out=ms.t[:, :]),
                 r=[src], w=[junk, ms])
            k.op('act', lambda e: e.activation(out=ms.t[:, :], in_=ms.t[:, :], func=AF.Ln,
                                               bias=epsb.t[:, :], scale=1.0), r=[epsb], w=[ms])
            k.op('act', lambda e: e.activation(out=rstd.t[:, :], in_=ms.t[:, :], func=AF.Exp, scale=-0.5),
                 r=[ms], w=[rstd])

        neghalf = k.sb(es, "neghalf", [128, 1], F32)
        ebase = k.sb(es, "ebase", [128, 128], F32)
        k.op('dve', lambda e: e.memset(neghalf.t[:, :], -0.5), w=[neghalf])
        k.op('dve', lambda e: e.memset(ebase.t[:, :], float(np.e)), w=[ebase])

        def rstd_pow(src, n_feat, ms, rstd, junk):
            k.op('act', lambda e: e.activation(out=junk.t[:, :], in_=src.t[:, :], func=AF.Square,
                                               scale=float(n_feat) ** -0.5, accum_out=ms.t[:, :]),
                 r=[src], w=[junk, ms])
            k.op('pool', lambda e: e.tensor_scalar(out=ms.t[:, :], in0=ms.t[:, :], scalar1=EPS, scalar2=None,
                                                   op0=ALU.add), w=[ms])
            k.op('pool', lambda e: e.tensor_tensor(out=rstd.t[:, :], in0=ms.t[:, :], in1=neghalf.t[:, :], op=ALU.pow),
                 r=[ms, neghalf], w=[rstd])

        class PeerState:
            pass

        def peer_alloc(pes):
            st = PeerState()
            st.iota = k.sb(pes, "iota", [128, 2048], F32)
            k.dma('sp', 'iota', lambda e: e.dma_start(out=st.iota.t[:, :], in_=iota_d[:, :]), w=[st.iota])
            st.wq = k.sb(pes, "wq_sb", [128, 8, 2048], BF16)
            st.keysT = k.sb(pes, "keysT_sb", [128, 2048], BF16)
            st.junk = k.sb(pes, "pjunk", [128, D], F32)
            st.ms = k.sb(pes, "pms", [128, 1], F32)
            st.rstd = k.sb(pes, "prstd", [128, 1], F32)
            st.hnb = [k.sb(pes, "hnb%d" % i, [128, D], BF16) for i in range(2)]
            st.hnT = k.sb(pes, "hnT", [128, D], BF16)
            st.qT = k.sb(pes, "qT", [128, 2048], BF16)
            st.scr = k.sb(pes, "scr", [128, 2048], F32)
            st.top = k.sb(pes, "top", [128, 256], F32)
            st.idx = k.sb(pes, "idx", [128, 256], U32)
            st.idxf = k.sb(pes, "idxf", [128, 256], F32)
            st.cand = k.sb(pes, "cand", [128, 2048], F32)
            st.best = k.sb(pes, "best", [128, 128], F32)
            st.pos = k.sb(pes, "pos", [128, 128], U32)
            st.posi = k.sb(pes, "posi", [128, 128], U32)
            st.posj = k.sb(pes, "posj", [128, 128], U32)
            st.pif = k.sb(pes, "pif", [128, 128], F32)
            st.pjf = k.sb(pes, "pjf", [128, 128], F32)
            st.oh = st.scr
            st.ea = k.sb(pes, "ea", [128, 128], F32)
            st.eb = k.sb(pes, "eb", [128, 128], F32)
            st.ef = k.sb(pes, "ef", [128, 128], F32)
            st.ssum = k.sb(pes, "ssum", [128, 8], F32)
            st.gate = [k.sb(pes, "gate%d" % i, [128, 128], F32) for i in range(2)]
            st.eidx = [k.sb(pes, "eidx%d" % i, [128, 128], U32) for i in range(2)]
            st.act = [k.sb(pes, "pact%d" % i, [128, 1], F32) for i in range(4)]
            st.gel = [k.sb(pes, "pgel%d" % i, [128, 1], F32) for i in range(4)]
            st.prod = [k.sb(pes, "prod%d" % i, [128, D], BF16) for i in range(3)]
            st.slots = [k.sb(pes, "gs%d" % i, [128, 2 * D], BF16) for i in range(NB)]
            st.wv = [k.sb(pes, "pwv%d" % i, [128, 1], F32) for i in range(4)]
            st.vs = [k.sb(pes, "pvs%d" % i, [128, D], BF16) for i in range(3)]
            st.gcount = 0
            return st

        def peer_load_weights(st, l):
            for c in range(8):
                k.dma('pool', 'wq%d' % (c % 2),
                      lambda e, c=c: e.dma_start(out=st.wq.t[:, c, :], in_=wq_d[l, c * 128:(c + 1) * 128, :]),
                      pw=[st.wq])
            k.dma('pool', 'keysT', lambda e: e.dma_start(out=st.keysT.t[:, :], in_=keysT_d[l, :, :]),
                  w=[st.keysT])

        def peer_prep(st, h, gain_ap, gain_buf, par):
            hnb, gate, eidx = st.hnb[par], st.gate[par], st.eidx[par]
            rstd_pow(h, D, st.ms, st.rstd, st.junk)
            k.op('dve', lambda e: e.scalar_tensor_tensor(out=hnb.t[:, :], in0=h.t[:, :], scalar=st.rstd.t[:, :],
                                                         in1=gain_ap, op0=ALU.mult, op1=ALU.mult),
                 r=[h, st.rstd, gain_buf], w=[hnb])
            pT = ps[0]
            pTb = pT.t[:, :].bitcast(BF16)
            for c in range(8):
                k.op('pe', lambda e, c=c: e.transpose(out=pTb[:, c * 128:(c + 1) * 128],
                                                      in_=hnb.t[:, c * 128:(c + 1) * 128],
                                                      identity=ident_b.t[:, :]),
                     r=[hnb, ident_b], **({'w': [pT]} if c == 0 else {'pw': [pT]}))
            k.op('act', lambda e: e.activation(out=st.hnT.t[:, :], in_=pTb, func=AF.Copy), r=[pT], w=[st.hnT])
            for bnk in range(4):
                pq = ps[1 + bnk % 2]
                for jj in range(4):
                    j = bnk * 4 + jj
                    for c in range(8):
                        first = (jj == 0 and c == 0)
                        k.op('pe', lambda e, j=j, jj=jj, c=c, pq=pq: e.matmul(
                            pq.t[:, jj * 128:(jj + 1) * 128],
                            lhsT=st.wq.t[:, c, j * 128:(j + 1) * 128],
                            rhs=st.hnT.t[:, c * 128:(c + 1) * 128],
                            start=(c == 0), stop=(c == 7)),
                             r=[st.wq, st.hnT], **({'w': [pq]} if first else {'pw': [pq]}))
                k.op('act', lambda e, bnk=bnk, pq=pq: e.activation(
                    out=st.qT.t[:, bnk * 512:(bnk + 1) * 512], in_=pq.t[:, :], func=AF.Copy),
                     r=[pq], **({'w': [st.qT]} if bnk == 0 else {'pw': [st.qT]}))

            def scores(bnk):
                pq = ps[1 + bnk % 2]
                for jj in range(4):
                    g = bnk * 4 + jj
                    k.op('pe', lambda e, g=g, jj=jj, pq=pq: e.matmul(
                        pq.t[:, jj * 128:(jj + 1) * 128],
                        lhsT=st.qT.t[:, g * 128:(g + 1) * 128],
                        rhs=st.keysT.t[:, g * 128:(g + 1) * 128],
                        start=True, stop=True),
                         r=[st.qT, st.keysT], **({'w': [pq]} if jj == 0 else {'pw': [pq]}))

            def topk1(bnk):
                pq = ps[1 + bnk % 2]
                for g in range(bnk * 4, bnk * 4 + 4):
                    sg = pq.t[:, (g % 4) * 128:(g % 4 + 1) * 128]
                    t0 = st.top.t[:, g * 16:g * 16 + 8]
                    t1 = st.top.t[:, g * 16 + 8:g * 16 + 16]
                    scr = st.scr.t[:, g * 128:(g + 1) * 128]
                    k.op('dve', lambda e, sg=sg, t0=t0: e.max(out=t0, in_=sg), r=[pq], pw=[st.top])
                    k.op('dve', lambda e, sg=sg, t0=t0, scr=scr: e.match_replace(
                        out=scr, in_to_replace=t0, in_values=sg, imm_value=NEG), r=[pq, st.top], pw=[st.scr])
                    k.op('dve', lambda e, t1=t1, scr=scr: e.max(out=t1, in_=scr), r=[st.scr], pw=[st.top])
                    k.op('dve', lambda e, g=g, sg=sg, t0=t0: e.max_index(
                        out=st.idx.t[:, g * 16:g * 16 + 8], in_max=t0, in_values=sg), r=[pq, st.top], pw=[st.idx])
                    k.op('dve', lambda e, g=g, sg=sg, t1=t1: e.max_index(
                        out=st.idx.t[:, g * 16 + 8:g * 16 + 16], in_max=t1, in_values=sg), r=[pq, st.top], pw=[st.idx])

            scores(0)
            scores(1)
            topk1(0)
            scores(2)
            topk1(1)
            scores(3)
            topk1(2)
            topk1(3)
            k.op('dve', lambda e: e.tensor_copy(out=st.idxf.t[:, :], in_=st.idx.t[:, :]), r=[st.idx], w=[st.idxf])
            top4 = st.top.t[:, :].rearrange("p (h two i) -> p h two i", h=8, two=2)
            in0 = top4[:, :, 0, :].unsqueeze(3).to_broadcast([128, 8, 16, 16])
            in1 = top4[:, :, 1, :].unsqueeze(2).to_broadcast([128, 8, 16, 16])
            cand4 = st.cand.t[:, :].rearrange("p (h i j) -> p h i j", h=8, i=16)
            k.op('dve', lambda e: e.tensor_tensor(out=cand4, in0=in0, in1=in1, op=ALU.add), r=[st.top], w=[st.cand])
            for hh in range(8):
                cg = st.cand.t[:, hh * 256:(hh + 1) * 256]
                b0 = st.best.t[:, hh * 16:hh * 16 + 8]
                b1 = st.best.t[:, hh * 16 + 8:hh * 16 + 16]
                scr = st.scr.t[:, hh * 256:(hh + 1) * 256]
                k.op('dve', lambda e, cg=cg, b0=b0: e.max(out=b0, in_=cg), r=[st.cand], pw=[st.best])
                k.op('dve', lambda e, cg=cg, b0=b0, scr=scr: e.match_replace(
                    out=scr, in_to_replace=b0, in_values=cg, imm_value=NEG), r=[st.cand, st.best], pw=[st.scr])
                k.op('dve', lambda e, b1=b1, scr=scr: e.max(out=b1, in_=scr), r=[st.scr], pw=[st.best])
                k.op('dve', lambda e, hh=hh, cg=cg, b0=b0: e.max_index(
                    out=st.pos.t[:, hh * 16:hh * 16 + 8], in_max=b0, in_values=cg), r=[st.cand, st.best], pw=[st.pos])
                k.op('dve', lambda e, hh=hh, cg=cg, b1=b1: e.max_index(
                    out=st.pos.t[:, hh * 16 + 8:hh * 16 + 16], in_max=b1, in_values=cg), r=[st.cand, st.best], pw=[st.pos])
            best3 = st.best.t[:, :].rearrange("p (h k) -> p h k", h=8)
            gate3 = gate.t[:, :].rearrange("p (h k) -> p h k", h=8)
            k.op('dve', lambda e: e.tensor_tensor(out=gate3, in0=best3,
                                                  in1=best3[:, :, 0:1].to_broadcast([128, 8, 16]), op=ALU.subtract),
                 r=[st.best], w=[gate])
            k.op('act', lambda e: e.activation(out=gate.t[:, :], in_=gate.t[:, :], func=AF.Exp), r=[gate], w=[gate])
            k.op('dve', lambda e: e.tensor_reduce(out=st.ssum.t[:, :], in_=gate3, axis=AX.X, op=ALU.add),
                 r=[gate], w=[st.ssum])
            k.op('dve', lambda e: e.reciprocal(out=st.ssum.t[:, :], in_=st.ssum.t[:, :]), r=[st.ssum], w=[st.ssum])
            k.op('dve', lambda e: e.tensor_tensor(out=gate3, in0=gate3,
                                                  in1=st.ssum.t[:, :].unsqueeze(2).to_broadcast([128, 8, 16]),
                                                  op=ALU.mult), r=[gate, st.ssum], w=[gate])
            k.op('dve', lambda e: e.tensor_scalar(out=st.posi.t[:, :], in0=st.pos.t[:, :], scalar1=4, scalar2=None,
                                                  op0=ALU.logical_shift_right), r=[st.pos], w=[st.posi])
            k.op('dve', lambda e: e.tensor_scalar(out=st.posj.t[:, :], in0=st.pos.t[:, :], scalar1=15, scalar2=None,
                                                  op0=ALU.bitwise_and), r=[st.pos], w=[st.posj])
            k.op('dve', lambda e: e.tensor_copy(out=st.pif.t[:, :], in_=st.posi.t[:, :]), r=[st.posi], w=[st.pif])
            k.op('dve', lambda e: e.tensor_copy(out=st.pjf.t[:, :], in_=st.posj.t[:, :]), r=[st.posj], w=[st.pjf])
            iota4 = st.iota.t[:, :].rearrange("p (h k i) -> p h k i", h=8, k=16)
            oh4 = st.oh.t[:, :].rearrange("p (h k i) -> p h k i", h=8, k=16)
            idx4 = st.idxf.t[:, :].rearrange("p (h two i) -> p h two i", h=8, two=2)
            for which, pf, dst in ((0, st.pif, st.ea), (1, st.pjf, st.eb)):
                pf4 = pf.t[:, :].rearrange("p (h k) -> p h k", h=8).unsqueeze(3).to_broadcast([128, 8, 16, 16])
                k.op('dve', lambda e, pf4=pf4: e.tensor_tensor(out=oh4, in0=iota4, in1=pf4, op=ALU.is_equal),
                     r=[st.iota, pf], w=[st.oh])
                ix = idx4[:, :, which, :].unsqueeze(2).to_broadcast([128, 8, 16, 16])
                k.op('dve', lambda e, ix=ix: e.tensor_tensor(out=oh4, in0=oh4, in1=ix, op=ALU.mult),
                     r=[st.oh, st.idxf], w=[st.oh])
                k.op('dve', lambda e, dst=dst: e.tensor_reduce(
                    out=dst.t[:, :], in_=st.oh.t[:, :].rearrange("p (a i) -> p a i", i=16), axis=AX.X, op=ALU.add),
                     r=[st.oh], w=[dst])
            k.op('dve', lambda e: e.scalar_tensor_tensor(out=st.ef.t[:, :], in0=st.ea.t[:, :], scalar=128.0,
                                                         in1=st.eb.t[:, :], op0=ALU.mult, op1=ALU.add),
                 r=[st.ea, st.eb], w=[st.ef])
            k.op('dve', lambda e: e.tensor_copy(out=eidx.t[:, :], in_=st.ef.t[:, :]), r=[st.ef], w=[eidx])

        class Pipe:
            def __init__(self):
                self.n = 0
                self.sched = {}

            def add(self, step, fn):
                self.sched.setdefault(step, []).append(fn)

            def run(self, step):
                for fn in self.sched.pop(step, []):
                    fn()

            def flush(self):
                for step in sorted(self.sched):
                    self.run(step)

        def peer_gather(pipe, st, l, h, par, ybanks, rec=None, fin=None):
            hnb, gate, eidx = st.hnb[par], st.gate[par], st.eidx[par]
            pa, pb = ybanks
            slot_of = {}

            def G(s_):
                slot = st.slots[st.gcount % NB]
                st.gcount += 1
                slot_of[s_] = slot
                k.dma('pool', 'g_' + slot.name, lambda e: e.indirect_dma_start(
                    out=slot.t[:, :], out_offset=None, in_=UVb[l][:, :],
                    in_offset=bass.IndirectOffsetOnAxis(ap=eidx.t[:, s_:s_ + 1], axis=0)),
                      r=[eidx, uvb[l]], w=[slot])

            def Dt(s_):
                slot = slot_of[s_]
                pr = st.prod[s_ % 3]
                k.op('dve', lambda e: e.tensor_tensor(out=pr.t[:, :], in0=slot.t[:, 0:D], in1=hnb.t[:, :], op=ALU.mult),
                     r=[slot, hnb], w=[pr])

            def P(s_):
                pr = st.prod[s_ % 3]
                pd = ps[3 + s_ % 2]
                for c in range(8):
                    k.op('pe', lambda e, c=c: e.matmul(
                        pd.t[:, 0:128], lhsT=ident_b.t[:, :], rhs=pr.t[:, c * 128:(c + 1) * 128],
                        start=(c == 0), stop=(c == 7)),
                         r=[ident_b, pr], **({'w': [pd]} if c == 0 else {'pw': [pd]}))

            def R(s_):
                pd = ps[3 + s_ % 2]
                a_ = st.act[s_ % 4]
                k.op('dve', lambda e: e.tensor_reduce(out=a_.t[:, :], in_=pd.t[:, 0:128], axis=AX.X, op=ALU.add),
                     r=[pd], w=[a_])

            def E(s_):
                a_, g_ = st.act[s_ % 4], st.gel[s_ % 4]
                k.op('act', lambda e: e.activation(out=g_.t[:, :], in_=a_.t[:, :], func=AF.Gelu), r=[a_], w=[g_])

            def X(s_):
                g_, w_ = st.gel[s_ % 4], st.wv[s_ % 4]
                k.op('dve', lambda e: e.tensor_tensor(out=w_.t[:, :], in0=g_.t[:, :], in1=gate.t[:, s_:s_ + 1],
                                                      op=ALU.mult), r=[g_, gate], w=[w_])

            def W(s_):
                slot, w_, vs = slot_of[s_], st.wv[s_ % 4], st.vs[s_ % 3]
                k.op('act', lambda e: e.activation(out=vs.t[:, :], in_=slot.t[:, D:2 * D], func=AF.Copy,
                                                   scale=w_.t[:, :]), r=[slot, w_], w=[vs])

            def M(s_):
                vs = st.vs[s_ % 3]
                for half, pb_ in ((0, pa), (1, pb)):
                    k.op('pe', lambda e, half=half, pb_=pb_: e.matmul(
                        pb_.t[:, :], lhsT=ident_b.t[:, :], rhs=vs.t[:, half * 512:(half + 1) * 512],
                        start=(s_ == 0), stop=(s_ == 127)),
                         r=[ident_b, vs], **({'w': [pb_]} if s_ == 0 else {'pw': [pb_]}))

            def final():
                for half, pb_ in ((0, pa), (1, pb)):
                    k.op('dve', lambda e, half=half, pb_=pb_: e.tensor_tensor(
                        out=h.t[:, half * 512:(half + 1) * 512], in0=h.t[:, half * 512:(half + 1) * 512],
                        in1=pb_.t[:, :], op=ALU.add), r=[pb_, h], pw=[h])
                if fin is not None:
                    fin()

            B = pipe.n
            for s_ in range(128):
                pipe.add(B + s_, lambda s_=s_: G(s_))
                pipe.add(B + s_, lambda s_=s_: Dt(s_))
                pipe.add(B + s_ + 1, lambda s_=s_: P(s_))
                pipe.add(B + s_ + 2, lambda s_=s_: R(s_))
                pipe.add(B + s_ + 3, lambda s_=s_: E(s_))
                pipe.add(B + s_ + 4, lambda s_=s_: X(s_))
                pipe.add(B + s_ + 5, lambda s_=s_: W(s_))
                pipe.add(B + s_ + 6, lambda s_=s_: M(s_))
            pipe.add(B + 127 + 6, final)
            for s_ in range(128):
                pipe.run(B + s_)
                if rec and s_ >= 8:
                    nd_left = sum(1 for r_ in rec if r_[0] == 'op' and r_[1][0] == 'dve')
                    left = max(1, 108 - s_)
                    k.replay(rec, max(8, (len(rec) + left - 1) // left + 4), ndve=min(4, max(1, (nd_left + left - 1) // left)))
                if rec and s_ == 120:
                    k.replay(rec, len(rec))
            if rec:
                k.replay(rec, len(rec))
            pipe.n = B + 128

        with ExitStack() as p1:
            gains = k.sb(p1, "gains", [128, 3, D], F32)
            for gi in range(3):
                k.dma('sp', 'gn%d' % gi, lambda e, gi=gi: e.dma_start(
                    out=gains.t[:, gi, :], in_=gains_d[gi:gi + 1, :].partition_broadcast(128)), pw=[gains])
            poolw = k.sb(p1, "poolw", [128, 8, 256], BF16)
            for g in range(4):
                for cc in range(2):
                    k.dma('pool', 'pw%d' % cc, lambda e, g=g, cc=cc: e.dma_start(
                        out=poolw.t[:, g * 2 + cc, :], in_=poolw_d[g, cc * 128:(cc + 1) * 128, :]), pw=[poolw])
            bands = k.sb(p1, "bands", [128, 12, 128], BF16)
            k.dma('pool', 'bands', lambda e: e.dma_start(out=bands.t[:, :, :], in_=bands_d[:, :, :]), w=[bands])
            st = peer_alloc(p1)
            peer_load_weights(st, 0)
            xt = [k.sb(p1, "xt%d" % i, [128, D], F32) for i in range(3)]
            hn = [k.sb(p1, "hn%d" % i, [128, D], BF16) for i in range(2)]
            pooledT = k.sb(p1, "pooledT", [128, D], BF16)
            ms1 = k.sb(p1, "ms1", [128, 1], F32)
            rstd1 = k.sb(p1, "rstd1", [128, 1], F32)
            mixs = k.sb(p1, "mixs", [128, D], F32)

            def load1(i):
                x_ = xt[i % 3]
                k.dma('sp', 'x%d' % (i % 3), lambda e: e.dma_start(out=x_.t[:, :], in_=xpad[i * 128:(i + 1) * 128, :]),
                      w=[x_])

            def front1(i):
                par = i % 2
                x_ = xt[i % 3]
                rstd_pow(x_, D, ms1, rstd1, st.junk)
                k.op('dve', lambda e: e.scalar_tensor_tensor(out=hn[par].t[:, :], in0=x_.t[:, :], scalar=rstd1.t[:, :],
                                                             in1=gains.t[:, 0, :], op0=ALU.mult, op1=ALU.mult),
                     r=[x_, rstd1, gains], w=[hn[par]])
                for bnk in range(2):
                    pp = ps[(0, 5)[bnk]]
                    for cc in range(4):
                        c = bnk * 4 + cc
                        g = c // 2
                        bcur = (8 + g) if i == 0 else g
                        k.op('pe', lambda e, c=c, cc=cc, pp=pp, bcur=bcur: e.matmul(
                            pp.t[:, cc * 128:(cc + 1) * 128], lhsT=hn[par].t[:, c * 128:(c + 1) * 128],
                            rhs=bands.t[:, bcur, :], start=True, stop=(i == 0)),
                             r=[hn[par], bands], **({'w': [pp]} if cc == 0 else {'pw': [pp]}))
                        if i > 0:
                            k.op('pe', lambda e, c=c, cc=cc, pp=pp, g=g: e.matmul(
                                pp.t[:, cc * 128:(cc + 1) * 128], lhsT=hn[1 - par].t[:, c * 128:(c + 1) * 128],
                                rhs=bands.t[:, 4 + g, :], start=False, stop=True),
                                 r=[hn[1 - par], bands], pw=[pp])
                    k.op('act', lambda e, bnk=bnk, pp=pp: e.activation(
                        out=pooledT.t[:, bnk * 512:(bnk + 1) * 512], in_=pp.t[:, :], func=AF.Copy),
                         r=[pp], **({'w': [pooledT]} if bnk == 0 else {'pw': [pooledT]}))
                for bnk in range(2):
                    pp = ps[(0, 5)[bnk]]
                    for gg in range(2):
                        g = bnk * 2 + gg
                        for cc in range(2):
                            c = g * 2 + cc
                            k.op('pe', lambda e, c=c, gg=gg, cc=cc, pp=pp: e.matmul(
                                pp.t[:, gg * 256:(gg + 1) * 256], lhsT=pooledT.t[:, c * 128:(c + 1) * 128],
                                rhs=poolw.t[:, c, :], start=(cc == 0), stop=(cc == 1)),
                                 r=[pooledT, poolw], **({'w': [pp]} if (gg == 0 and cc == 0) else {'pw': [pp]}))
                    sl = slice(bnk * 512, (bnk + 1) * 512)
                    k.op('dve', lambda e, pp=pp, sl=sl: e.tensor_tensor(
                        out=mixs.t[:, sl], in0=pp.t[:, :], in1=gains.t[:, 2, sl], op=ALU.mult),
                         r=[pp, gains], **({'w': [mixs]} if bnk == 0 else {'pw': [mixs]}))
                k.op('dve', lambda e: e.tensor_tensor(out=x_.t[:, :], in0=x_.t[:, :], in1=mixs.t[:, :], op=ALU.add),
                     r=[mixs], w=[x_])
                peer_prep(st, x_, gains.t[:, 1, :], gains, par)

            pipe1 = Pipe()

            def back1(i, rec=None):
                par = i % 2
                x_ = xt[i % 3]
                peer_gather(pipe1, st, 0, x_, par, (ps[6], ps[7]), rec,
                            fin=lambda: k.dma('sp', 'h2st%d' % par, lambda e: e.dma_start(
                                out=H2[i * 128:(i + 1) * 128, :], in_=x_.t[:, :]), r=[x_]))

            load1(0)
            if NT > 1:
                load1(1)
            front1(0)
            for i in range(NT):
                k.rec = rec = []
                if i + 2 < NT:
                    load1(i + 2)
                if i + 1 < NT:
                    front1(i + 1)
                k.rec = None
                back1(i, rec)
            pipe1.flush()
            k.barrier()

        with ExitStack() as p2:
            gains2 = k.sb(p2, "gains2", [128, 2, D], F32)
            for gi, src in ((0, 3), (1, 4)):
                k.dma('sp', 'gn%d' % gi, lambda e, gi=gi, src=src: e.dma_start(
                    out=gains2.t[:, gi, :], in_=gains_d[src:src + 1, :].partition_broadcast(128)), pw=[gains2])
            hnm = k.sb(p2, "hnm", [128, 2, 64], F32)
            for gi in range(2):
                k.dma('sp', 'hn%d' % gi, lambda e, gi=gi: e.dma_start(
                    out=hnm.t[:, gi, :], in_=hnorm_d[gi:gi + 1, :].partition_broadcast(128)), pw=[hnm])
            k.op('dve', lambda e: e.tensor_scalar(out=hnm.t[:, 1, :], in0=hnm.t[:, 1, :], scalar1=0.125, scalar2=None,
                                                  op0=ALU.mult), r=[hnm], w=[hnm])
            wkv = k.sb(p2, "wkv", [128, 8, 2048], BF16)
            wqa = k.sb(p2, "wqa", [128, 8, D], BF16)
            for c in range(8):
                k.dma('pool', 'wq%d' % (c % 2), lambda e, c=c: e.dma_start(
                    out=wkv.t[:, c, :], in_=wkv_d[c * 128:(c + 1) * 128, :]), pw=[wkv])
                k.dma('pool', 'pw%d' % (c % 2), lambda e, c=c: e.dma_start(
                    out=wqa.t[:, c, :], in_=wqa_d[c * 128:(c + 1) * 128, :]), pw=[wqa])
            ht = [k.sb(p2, "ht%d" % i, [128, D], F32) for i in range(2)]
            junk2 = k.sb(p2, "junk2", [128, D], F32)
            ms2 = k.sb(p2, "ms2", [128, 1], F32)
            rstd2 = k.sb(p2, "rstd2", [128, 1], F32)
            nb_ = [k.sb(p2, "nb%d" % i, [128, D], BF16) for i in range(2)]
            nT = [k.sb(p2, "nT%d" % i, [128, D], BF16) for i in range(2)]
            sq = k.sb(p2, "sq", [128, D], F32)
            ss = k.sb(p2, "ss", [128, 16], F32)
            tmpn = k.sb(p2, "tmpn", [128, D], F32)
            knb = k.sb(p2, "knb", [128, D], BF16)
            kT = [k.sb(p2, "kT%d" % i, [128, D], BF16) for i in range(2)]
            vb = [k.sb(p2, "vb%d" % i, [128, D], BF16) for i in range(2)]

            def headnorm_T(src_banks, gi, dstT, dram, i, tag):
                for half, pb_ in enumerate(src_banks):
                    sl = slice(half * 512, (half + 1) * 512)
                    k.op('act', lambda e, pb_=pb_, sl=sl: e.activation(out=sq.t[:, sl], in_=pb_.t[:, :], func=AF.Square),
                         r=[pb_], **({'w': [sq]} if half == 0 else {'pw': [sq]}))
                k.op('dve', lambda e: e.tensor_reduce(out=ss.t[:, :], in_=sq.t[:, :].rearrange("p (h d) -> p h d", d=64),
                                                      axis=AX.X, op=ALU.add), r=[sq], w=[ss])
                k.op('act', lambda e: e.activation(out=ss.t[:, :], in_=ss.t[:, :], func=AF.Ln, bias=epsb.t[:, :],
                                                   scale=1.0 / 64), r=[ss, epsb], w=[ss])
                k.op('act', lambda e: e.activation(out=ss.t[:, :], in_=ss.t[:, :], func=AF.Exp, scale=-0.5),
                     r=[ss], w=[ss])
                for half, pb_ in enumerate(src_banks):
                    sl = slice(half * 512, (half + 1) * 512)
                    k.op('dve', lambda e, pb_=pb_, sl=sl, half=half: e.tensor_tensor(
                        out=tmpn.t[:, sl].rearrange("p (h d) -> p h d", d=64),
                        in0=pb_.t[:, :].rearrange("p (h d) -> p h d", d=64),
                        in1=ss.t[:, half * 8:(half + 1) * 8].unsqueeze(2).to_broadcast([128, 8, 64]), op=ALU.mult),
                         r=[pb_, ss], **({'w': [tmpn]} if half == 0 else {'pw': [tmpn]}))
                k.op('dve', lambda e: e.tensor_tensor(
                    out=knb.t[:, :].rearrange("p (h d) -> p h d", d=64),
                    in0=tmpn.t[:, :].rearrange("p (h d) -> p h d", d=64),
                    in1=hnm.t[:, gi, :].unsqueeze(1).to_broadcast([128, 16, 64]), op=ALU.mult),
                     r=[tmpn, hnm], w=[knb])
                pT = src_banks[0]
                pTb = pT.t[:, :].bitcast(BF16)
                for c in range(8):
                    k.op('pe', lambda e, c=c: e.transpose(out=pTb[:, c * 128:(c + 1) * 128],
                                                          in_=knb.t[:, c * 128:(c + 1) * 128], identity=ident_b.t[:, :]),
                         r=[knb, ident_b], **({'w': [pT]} if c == 0 else {'pw': [pT]}))
                k.op('act', lambda e: e.activation(out=dstT.t[:, :], in_=pTb, func=AF.Copy), r=[pT], w=[dstT])
                for hh in range(2):
                    k.dma('sp', '%s%d_%d' % (tag, i % 2, hh), lambda e, hh=hh: e.dma_start(
                        out=dram.rearrange("(pr two) d l -> two d pr l", two=2)[hh, :, :, i * 128:(i + 1) * 128],
                        in_=dstT.t[hh * 64:(hh + 1) * 64, :].rearrange("p (pr t) -> p pr t", t=128)),
                          r=[dstT])

            for i in range(NT):
                par = i % 2
                h_ = ht[par]
                k.dma('sp', 'x%d' % par, lambda e: e.dma_start(out=h_.t[:, :], in_=H2[i * 128:(i + 1) * 128, :]), w=[h_])
                rstd_of(h_, D, ms2, rstd2, junk2)
                for gi in range(2):
                    k.op('dve', lambda e, gi=gi: e.scalar_tensor_tensor(
                        out=nb_[gi].t[:, :], in0=h_.t[:, :], scalar=rstd2.t[:, :], in1=gains2.t[:, gi, :],
                        op0=ALU.mult, op1=ALU.mult), r=[h_, rstd2, gains2], w=[nb_[gi]])
                    pT = ps[gi]
                    pTb = pT.t[:, :].bitcast(BF16)
                    for c in range(8):
                        k.op('pe', lambda e, c=c, gi=gi, pTb=pTb: e.transpose(
                            out=pTb[:, c * 128:(c + 1) * 128], in_=nb_[gi].t[:, c * 128:(c + 1) * 128],
                            identity=ident_b.t[:, :]),
                             r=[nb_[gi], ident_b], **({'w': [pT]} if c == 0 else {'pw': [pT]}))
                    k.op('act', lambda e, gi=gi, pTb=pTb: e.activation(out=nT[gi].t[:, :], in_=pTb, func=AF.Copy),
                         r=[pT], w=[nT[gi]])
                for blk in range(4):
                    pb_ = ps[2 + blk]
                    for c in range(8):
                        k.op('pe', lambda e, c=c, blk=blk, pb_=pb_: e.matmul(
                            pb_.t[:, :], lhsT=nT[0].t[:, c * 128:(c + 1) * 128],
                            rhs=wkv.t[:, c, blk * 512:(blk + 1) * 512], start=(c == 0), stop=(c == 7)),
                             r=[nT[0], wkv], **({'w': [pb_]} if c == 0 else {'pw': [pb_]}))
                for blk in range(2):
                    pb_ = ps[6 + blk]
                    for c in range(8):
                        k.op('pe', lambda e, c=c, blk=blk, pb_=pb_: e.matmul(
                            pb_.t[:, :], lhsT=nT[1].t[:, c * 128:(c + 1) * 128],
                            rhs=wqa.t[:, c, blk * 512:(blk + 1) * 512], start=(c == 0), stop=(c == 7)),
                             r=[nT[1], wqa], **({'w': [pb_]} if c == 0 else {'pw': [pb_]}))
                v_ = vb[par]
                for half in range(2):
                    k.op('act', lambda e, half=half: e.activation(
                        out=v_.t[:, half * 512:(half + 1) * 512], in_=ps[4 + half].t[:, :], func=AF.Copy),
                         r=[ps[4 + half]], **({'w': [v_]} if half == 0 else {'pw': [v_]}))
                k.dma('sp', 'vst%d' % par, lambda e: e.dma_start(out=VV[i * 128:(i + 1) * 128, :], in_=v_.t[:, :]), r=[v_])
                headnorm_T((ps[2], ps[3]), 0, kT[0], KT, i, 'kst')
                headnorm_T((ps[6], ps[7]), 1, kT[1], QT, i, 'qst')
            k.barrier()

        with ExitStack() as p3:
            kth = [k.sb(p3, "kth%d" % i, [64, Lp], BF16) for i in range(2)]
            qth = [k.sb(p3, "qth%d" % i, [64, Lp], BF16) for i in range(2)]
            vh = [k.sb(p3, "vh%d" % i, [128, NT, 64], BF16) for i in range(2)]
            e_sb = [k.sb(p3, "e_sb%d" % i, [128, 512], F32) for i in range(2)]
            sp_sb = [k.sb(p3, "sp_sb%d" % i, [128, 512], BF16) for i in range(3)]
            wg_sb = [k.sb(p3, "wg_sb%d" % i, [128, 512], BF16) for i in range(3)]
            S = [k.sb(p3, "S%d" % i, [128, 512], BF16) for i in range(2)]
            o_sb = [k.sb(p3, "o_sb%d" % i, [64, 512], BF16) for i in range(2)]
            convert_tables(1)
            one = k.sb(p3, "onec", [128, 1], F32)
            k.op('dve', lambda e: e.memset(one.t[:, :], 1.0), w=[one])
            NA = 5
            tasks = []

            def load_head(head):
                hp = head % 2
                k.dma('sp', 'kth%d' % hp, lambda e: e.dma_start(out=kth[hp].t[:, :], in_=KT[head, :, :]), w=[kth[hp]])
                k.dma('sp', 'qth%d' % hp, lambda e: e.dma_start(out=qth[hp].t[:, :], in_=QT[head, :, :]), w=[qth[hp]])
                k.dma('sp', 'vh%d' % hp, lambda e: e.dma_start(
                    out=vh[hp].t[:, :, :],
                    in_=VV.rearrange("(j p) d -> p j d", p=128)[:, :, head * 64:(head + 1) * 64]), w=[vh[hp]])

            def make_task(t, head, qa, qb, j, n, g):
                hp = head % 2
                ncols = (qb - qa + 1) * 128
                cs = max(j - qa, 0) * 128
                A = ps[t % NA]
                eb_ = e_sb[t % 2]
                spb = sp_sb[t % 3]
                wgb = wg_sb[t % 3]
                O = ps[NA + g % 2]
                osb = o_sb[g % 2]
                diag = j >= qa
                first = (n == 0)
                last = (j == 0)
                Sold = S[(n - 1) % 2]
                Snew = S[n % 2]

                def s1():
                    if first and qa == 1 and head + 1 < 16:
                        load_head(head + 1)
                    if first:
                        k.op('dve', lambda e: e.memset(O.t[0:64, :], 0.0), w=[O])
                    k.op('pe', lambda e: e.matmul(
                        A.t[:, cs:ncols], lhsT=kth[hp].t[:, j * 128:(j + 1) * 128],
                        rhs=qth[hp].t[:, qa * 128 + cs:qa * 128 + ncols], start=True, stop=False),
                         r=[kth[hp], qth[hp]], w=[A])

                def s2():
                    k.op('act', lambda e: e.activation(
                        out=eb_.t[:, cs:ncols], in_=A.t[:, cs:ncols], func=AF.Exp), r=[A], w=[eb_])
                    k.op('act', lambda e: e.activation(
                        out=spb.t[:, cs:ncols], in_=eb_.t[:, cs:ncols], func=AF.Ln, bias=one.t[:, :], scale=1.0),
                         r=[eb_, one], w=[spb])
                    if diag:
                        k.op('dve', lambda e: e.tensor_tensor(
                            out=spb.t[:, cs:cs + 128], in0=spb.t[:, cs:cs + 128], in1=trim.t[:, :], op=ALU.mult),
                             r=[spb, trim], w=[spb])
                    if j == 0:
                        k.op('dve', lambda e: e.tensor_scalar(
                            out=spb.t[:, cs:ncols], in0=spb.t[:, cs:ncols], scalar1=rowm.t[:, :], scalar2=None,
                            op0=ALU.mult), r=[spb, rowm], w=[spb])

                def s3():
                    k.op('pe', lambda e: e.matmul(
                        A.t[:, cs:ncols], lhsT=negtri.t[:, :], rhs=spb.t[:, cs:ncols], start=False, stop=first),
                         r=[negtri, spb], pw=[A])
                    if not first:
                        k.op('pe', lambda e: e.matmul(
                            A.t[:, cs:ncols], lhsT=negones.t[:, :], rhs=Sold.t[:, cs:ncols], start=False, stop=True),
                             r=[negones, Sold], pw=[A])
                    if j > 0:
                        if first:
                            k.op('dve', lambda e: e.memset(Snew.t[:, :], 0.0), w=[Snew])
                            k.op('dve', lambda e: e.tensor_copy(
                                out=Snew.t[:, cs:ncols], in_=spb.t[:, cs:ncols]), r=[spb], w=[Snew])
                        elif cs > 0:
                            k.op('dve', lambda e: e.memset(Snew.t[:, 0:cs], 0.0), w=[Snew])
                            k.op('dve', lambda e: e.tensor_tensor(
                                out=Snew.t[:, cs:ncols], in0=Sold.t[:, cs:ncols], in1=spb.t[:, cs:ncols],
                                op=ALU.add), r=[Sold, spb], pw=[Snew])
                        else:
                            k.op('dve', lambda e: e.tensor_tensor(
                                out=Snew.t[:, 0:ncols], in0=Sold.t[:, 0:ncols], in1=spb.t[:, 0:ncols],
                                op=ALU.add), r=[Sold, spb], w=[Snew])

                def s4():
                    k.op('act', lambda e: e.activation(
                        out=wgb.t[:, cs:ncols], in_=A.t[:, cs:ncols], func=AF.Exp), r=[A], w=[wgb])
                    if diag:
                        k.op('dve', lambda e: e.tensor_tensor(
                            out=wgb.t[:, cs:cs + 128], in0=wgb.t[:, cs:cs + 128], in1=trim.t[:, :], op=ALU.mult),
                             r=[wgb, trim], w=[wgb])
                    if j == 0:
                        k.op('dve', lambda e: e.tensor_scalar(
                            out=wgb.t[:, cs:ncols], in0=wgb.t[:, cs:ncols], scalar1=rowm.t[:, :], scalar2=None,
                            op0=ALU.mult), r=[wgb, rowm], w=[wgb])

                def s5():
                    k.op('pe', lambda e: e.matmul(
                        O.t[0:64, cs:ncols], lhsT=vh[hp].t[:, j, :], rhs=wgb.t[:, cs:ncols],
                        start=False, stop=last, skip_group_check=True),
                         r=[vh[hp], wgb], pw=[O])
                    if last:
                        k.op('act', lambda e: e.activation(
                            out=osb.t[:, 0:ncols], in_=O.t[0:64, 0:ncols], func=AF.Copy), r=[O], w=[osb])
                        k.dma('sp', 'ost' + osb.name, lambda e: e.dma_start(
                            out=AT[head * 64:(head + 1) * 64, qa * 128:qa * 128 + ncols], in_=osb.t[:, 0:ncols]),
                              r=[osb])
                return (s1, s2, s3, s4, s5)

            t = 0
            g = 0
            for head in range(16):
                for qa in range(1, NT, 4):
                    qb = min(qa + 3, NT - 1)
                    for n, j in enumerate(range(qb, -1, -1)):
                        tasks.append(make_task(t, head, qa, qb, j, n, g))
                        t += 1
                    g += 1
            load_head(0)
            NTK = len(tasks)
            for step in range(NTK + 4):
                for st_i in (4, 3, 2, 1, 0):
                    ti = step - st_i
                    if 0 <= ti < NTK:
                        tasks[ti][st_i]()
            k.barrier()

        with ExitStack() as p4:
            gains4 = k.sb(p4, "gains4", [128, D], F32)
            k.dma('sp', 'gn0', lambda e: e.dma_start(out=gains4.t[:, :], in_=gains_d[5:6, :].partition_broadcast(128)),
                  w=[gains4])
            wo = k.sb(p4, "wo", [128, 8, D], BF16)
            for c in range(8):
                k.dma('pool', 'pw%d' % (c % 2), lambda e, c=c: e.dma_start(
                    out=wo.t[:, c, :], in_=wo_d[c * 128:(c + 1) * 128, :]), pw=[wo])
            st = peer_alloc(p4)
            peer_load_weights(st, 1)
            h4 = [k.sb(p4, "h4_%d" % i, [128, D], F32) for i in range(3)]
            att = [k.sb(p4, "att%d" % i, [128, 8, 128], BF16) for i in range(3)]

            def load4(i):
                h_, a_ = h4[i % 3], att[i % 3]
                k.dma('sp', 'x%d' % (i % 3), lambda e: e.dma_start(out=h_.t[:, :], in_=H2[i * 128:(i + 1) * 128, :]), w=[h_])
                k.dma('sp', 'att%d' % (i % 3), lambda e: e.dma_start(
                    out=a_.t[:, :, :],
                    in_=AT.rearrange("(c p) l -> p c l", p=128)[:, :, i * 128:(i + 1) * 128]), w=[a_])

            def front4(i):
                par = i % 2
                h_ = h4[i % 3]
                for half in range(2):
                    pb_ = ps[(0, 5)[half]]
                    for c in range(8):
                        k.op('pe', lambda e, c=c, half=half, pb_=pb_: e.matmul(
                            pb_.t[:, :], lhsT=att[i % 3].t[:, c, :], rhs=wo.t[:, c, half * 512:(half + 1) * 512],
                            start=(c == 0), stop=(c == 7)),
                             r=[att[i % 3], wo], **({'w': [pb_]} if c == 0 else {'pw': [pb_]}))
                    sl = slice(half * 512, (half + 1) * 512)
                    k.op('dve', lambda e, pb_=pb_, sl=sl: e.tensor_tensor(
                        out=h_.t[:, sl], in0=h_.t[:, sl], in1=pb_.t[:, :], op=ALU.add), r=[pb_, h_], pw=[h_])
                peer_prep(st, h_, gains4.t[:, :], gains4, par)

            pipe4 = Pipe()

            def back4(i, rec=None):
                par = i % 2
                h_ = h4[i % 3]
                peer_gather(pipe4, st, 1, h_, par, (ps[6], ps[7]), rec,
                            fin=lambda: k.dma('sp', 'h2st%d' % par, lambda e: e.dma_start(
                                out=out_d[(i - 1) * 128:i * 128, :], in_=h_.t[:, :]), r=[h_]))

            load4(1)
            if NT > 2:
                load4(2)
            front4(1)
            for i in range(1, NT):
                k.rec = rec = []
                if i + 2 < NT:
                    load4(i + 2)
                if i + 1 < NT:
                    front4(i + 1)
                k.rec = None
                back4(i, rec)
            pipe4.flush()
            k.barrier()
        print("build: ninst=%d" % k.ninst, {e: k.cnt[e] for e in COMPUTE})
    return nc


def _consts():
    ident = np.eye(128, dtype=np.float32)
    kk = np.arange(128)
    trim = (kk[:, None] < kk[None, :]).astype(np.float32)
    negtri = -(kk[:, None] >= kk[None, :]).astype(np.float32)
    negones = -np.ones((128, 128), np.float32)
    cst = np.stack([ident, trim, negtri, negones], axis=1)
    rowm = (kk >= 112).astype(np.float32)[:, None]
    iota = np.tile(np.arange(16, dtype=np.float32), 128)[None, :].repeat(128, axis=0)
    bands = np.zeros((128, 12, 128), np.float32)
    for g, w in enumerate((2, 4, 8, 16)):
        for t in range(128):
            for tp in range(t - w + 1, t + 1):
                if tp >= 0:
                    bands[tp, g, t] += 1.0 / w
                else:
                    bands[128 + tp, 4 + g, t] += 1.0 / w
            bands[t, g, t] -= 1.0
            l = t - 112
            c = float(min(max(l, 0) + 1, w))
            for tp in range(t - w + 1, t + 1):
                if tp >= 0:
                    bands[tp, 8 + g, t] += 1.0 / c
            bands[t, 8 + g, t] -= 1.0
    return cst, rowm, iota, bands


def _in_maps(inputs, NT, cores):
    x = np.asarray(inputs["x"], np.float32)
    meta = np.asarray(inputs["meta_tokens"], np.float32)
    Lp = NT * 128
    cst, rowm, iota, bands = _consts()
    gains = np.stack([inputs["norm_mix"][0], inputs["norm_ffn"][0], inputs["pool_scale"][0], inputs["kv_norm"],
                      inputs["norm_mix"][1], inputs["norm_ffn"][1]]).astype(np.float32)
    hnorm = np.stack([inputs["k_norm"], inputs["q_norm"][0]]).astype(np.float32)
    keysT = np.ascontiguousarray(np.transpose(np.asarray(inputs["peer_keys"], np.float32), (0, 4, 1, 2, 3))
                                 ).reshape(2, 128, 2048)
    shared = {
        "gains": np.ascontiguousarray(gains), "hnorm": np.ascontiguousarray(hnorm),
        "poolw": np.ascontiguousarray(inputs["pool_w"][0], dtype=np.float32), "bands": bands,
        "wq": np.ascontiguousarray(inputs["peer_wq"], dtype=np.float32), "keysT": keysT,
        "wkv": np.ascontiguousarray(inputs["w_kv"], dtype=np.float32),
        "wqa": np.ascontiguousarray(inputs["w_q"][0], dtype=np.float32),
        "wo": np.ascontiguousarray(inputs["w_o"][0], dtype=np.float32),
        "pu0": np.ascontiguousarray(inputs["peer_u"][0], dtype=np.float32),
        "pu1": np.ascontiguousarray(inputs["peer_u"][1], dtype=np.float32),
        "pv0": np.ascontiguousarray(inputs["peer_v"][0], dtype=np.float32),
        "pv1": np.ascontiguousarray(inputs["peer_v"][1], dtype=np.float32),
        "cst": cst, "rowm": rowm, "iota": iota,
    }
    maps = []
    for b in cores:
        xp = np.zeros((Lp, D), np.float32)
        xp[112:128] = meta
        xp[128:] = x[b, :Lp - 128]
        m = dict(shared)
        m["xpad"] = xp
        maps.append(m)
    return maps


def run(inputs, NT, cores, dbg=False):
    nc = build(NT, dbg=dbg)
    maps = _in_maps(inputs, NT, cores)
    res = run_bass_kernel_spmd(nc, maps, core_ids=list(range(len(cores))))
    return res


def kernel(**inputs):
    NT = 65
    res = run(inputs, NT, list(range(8)))
    return np.stack([np.asarray(r["out"], np.float32) for r in res.results], axis=0)
```

```python
import numpy as np
from contextlib import ExitStack
import concourse.bass as bass
import concourse.mybir as mybir
from concourse.alu_op_type import AluOpType as ALU
from concourse.bass_utils import run_bass_kernel_spmd

F32 = mybir.dt.float32
BF16 = mybir.dt.bfloat16
U32 = mybir.dt.uint32
I32 = mybir.dt.int32
AF = mybir.ActivationFunctionType
AX = mybir.AxisListType

D = 1024
EPS = 1e-6
NEG = -1.0e30
NB = 10
COMPUTE = ('pe', 'act', 'dve', 'pool')


class Buf:
    __slots__ = ('t', 'w', 'fw', 'r', 'name')

    def __init__(self, t, name=''):
        self.t = t
        self.w = {}
        self.fw = {}
        self.r = {}
        self.name = name


class K:
    def __init__(self, nc, es):
        self.nc = nc
        self.es = es
        self.engs = {'pe': nc.tensor, 'act': nc.scalar, 'dve': nc.vector,
                     'pool': nc.gpsimd, 'sp': nc.sync}
        self.semh = {}
        self.cnt = {}
        for e in COMPUTE:
            self.semh[e] = es.enter_context(nc.semaphore('sem_' + e))
            self.cnt[e] = 0
        self.seen = {e: {} for e in self.engs}
        self.ninst = 0

    def sb(self, es, name, shape, dt):
        self.nalloc = getattr(self, 'nalloc', 0) + 1
        return Buf(es.enter_context(self.nc.sbuf_tensor("s%d_%s" % (self.nalloc, name), list(shape), dt)), name)

    def _dsem(self, key):
        if key not in self.semh:
            self.semh[key] = self.es.enter_context(self.nc.semaphore('d_' + key))
            self.cnt[key] = 0
        return self.semh[key]

    def _wait(self, e, deps):
        seen = self.seen[e]
        for key, val in deps.items():
            if val <= 0 or seen.get(key, 0) >= val:
                continue
            self.engs[e].wait_ge(self.semh[key], val)
            seen[key] = val
            self.ninst += 1

    @staticmethod
    def _merge(d, s):
        for k_, v in s.items():
            if d.get(k_, 0) < v:
                d[k_] = v

    def _deps(self, r, w, pw):
        deps = {}
        for b in r:
            self._merge(deps, b.w)
        for b in w:
            self._merge(deps, b.w)
            self._merge(deps, b.r)
        for b in pw:
            self._merge(deps, b.fw)
            self._merge(deps, b.r)
        return deps

    def _mark(self, key, val, r, w, pw):
        for b in r:
            if b.r.get(key, 0) < val:
                b.r[key] = val
        for b in w:
            b.w = {key: val}
            b.fw = {key: val}
            b.r = {}
        for b in pw:
            if b.w.get(key, 0) < val:
                b.w[key] = val

    rec = None

    def replay(self, rec, n, ndve=None, nact=None, chain_break=False):
        saved, self.rec = self.rec, None
        nd = na = 0
        written = {}
        for _ in range(min(n, len(rec))):
            kind, args, kw = rec[0]
            eng = args[0] if kind == 'op' else 'dma'
            if ndve is not None and eng == 'dve':
                if nd >= ndve:
                    break
            if nact is not None and eng == 'act':
                if na >= nact:
                    break
            bufs = list(kw.get('r', ())) + list(kw.get('w', ())) + list(kw.get('pw', ()))
            if chain_break and any(id(b) in written and written[id(b)] != eng for b in bufs):
                break
            if eng == 'dve':
                nd += 1
            if eng == 'act':
                na += 1
            rec.pop(0)
            (self.op if kind == 'op' else self.dma)(*args, **kw)
            for b in list(kw.get('w', ())) + list(kw.get('pw', ())):
                written[id(b)] = eng
        self.rec = saved

    def op(self, e, fn, r=(), w=(), pw=()):
        if self.rec is not None:
            self.rec.append(('op', (e, fn), dict(r=r, w=w, pw=pw)))
            return
        self._wait(e, self._deps(r, w, pw))
        inst = fn(self.engs[e])
        self.cnt[e] += 1
        inst.then_inc(self.semh[e], 1)
        self.ninst += 1
        self._mark(e, self.cnt[e], r, w, pw)

    def dma(self, q, key, fn, r=(), w=(), pw=()):
        if self.rec is not None:
            self.rec.append(('dma', (q, key, fn), dict(r=r, w=w, pw=pw)))
            return
        self._dsem(key)
        deps = self._deps(r, w, pw)
        if self.cnt[key] > 0:
            deps[key] = max(deps.get(key, 0), self.cnt[key])
        self._wait(q, deps)
        inst = fn(self.engs[q])
        self.cnt[key] += 16
        inst.then_inc(self.semh[key], 16)
        self.ninst += 1
        self._mark(key, self.cnt[key], r, w, pw)

    def barrier(self):
        alld = {key: v for key, v in self.cnt.items() if v > 0}
        for e in self.engs:
            self._wait(e, alld)


def bcast_rows(ap2d_row, nparts):
    return ap2d_row.partition_broadcast(nparts)


def build(NT, dbg=False):
    nc = bass.Bass("TRN2", target_bir_lowering=False)
    Lp = NT * 128
    NQ = NT - 1
    din = lambda name, shape, dt=F32: nc.dram_tensor(name, list(shape), dt, kind="ExternalInput").ap()
    xpad = din("xpad", [Lp, D])
    gains_d = din("gains", [6, D])
    hnorm_d = din("hnorm", [2, 64])
    poolw_d = din("poolw", [4, 256, 256])
    bands_d = din("bands", [128, 12, 128])
    wq_d = din("wq", [2, D, 2048])
    keysT_d = din("keysT", [2, 128, 2048])
    wkv_d = din("wkv", [D, 2048])
    wqa_d = din("wqa", [D, D])
    wo_d = din("wo", [D, D])
    pu_d = [din("pu%d" % l, [16384, D]) for l in range(2)]
    pv_d = [din("pv%d" % l, [16384, D]) for l in range(2)]
    cst_d = din("cst", [128, 4, 128])
    rowm_d = din("rowm", [128, 1])
    iota_d = din("iota", [128, 2048])
    out_d = nc.dram_tensor("out", [NQ * 128, D], F32, kind="ExternalOutput").ap()
    skind = "ExternalOutput" if dbg else "Internal"
    H2 = nc.dram_tensor("H2", [Lp, D], F32, kind=skind).ap()
    KT = nc.dram_tensor("KT", [16, 64, Lp], BF16, kind=skind).ap()
    QT = nc.dram_tensor("QT", [16, 64, Lp], BF16, kind=skind).ap()
    VV = nc.dram_tensor("VV", [Lp, D], BF16, kind=skind).ap()
    AT = nc.dram_tensor("AT", [D, Lp], BF16, kind=skind).ap()
    UVb = [nc.dram_tensor("UVb%d" % l, [16384, 2 * D], BF16, kind="Internal").ap() for l in range(2)]

    with ExitStack() as es:
        k = K(nc, es)
        ps = [Buf(es.enter_context(nc.psum_tensor("ps%d" % i, [128, 512], F32)), "ps%d" % i)
              for i in range(8)]
        ident_b = k.sb(es, "ident_b", [128, 128], BF16)
        ident_f = k.sb(es, "ident_f", [128, 128], F32)
        trim = k.sb(es, "trim", [128, 128], BF16)
        negtri = k.sb(es, "negtri", [128, 128], BF16)
        negones = k.sb(es, "negones", [128, 128], BF16)
        rowm = k.sb(es, "rowm", [128, 1], F32)
        epsb = k.sb(es, "epsb", [128, 1], F32)
        k.dma('pool', 'c0', lambda e: e.dma_start(out=ident_b.t[:, :], in_=cst_d[:, 0, :]), w=[ident_b])
        k.dma('sp', 'c1', lambda e: e.dma_start(out=ident_f.t[:, :], in_=cst_d[:, 0, :]), w=[ident_f])
        k.dma('pool', 'c2', lambda e: e.dma_start(out=trim.t[:, :], in_=cst_d[:, 1, :]), w=[trim])
        k.dma('pool', 'c3', lambda e: e.dma_start(out=negtri.t[:, :], in_=cst_d[:, 2, :]), w=[negtri])
        k.dma('pool', 'c4', lambda e: e.dma_start(out=negones.t[:, :], in_=cst_d[:, 3, :]), w=[negones])
        k.dma('sp', 'c5', lambda e: e.dma_start(out=rowm.t[:, :], in_=rowm_d[:, :]), w=[rowm])
        k.op('dve', lambda e: e.memset(epsb.t[:, :], EPS), w=[epsb])

        uvb = [Buf(None, "uvb%d" % l) for l in range(2)]

        def convert_tables(l):
            for which, src in ((0, pu_d[l]), (1, pv_d[l])):
                for rr in range(4):
                    k.dma('pool', 'cv%d' % ((which * 4 + rr) % 4), lambda e, l=l, which=which, src=src, rr=rr: e.dma_start(
                        out=UVb[l][rr * 4096:(rr + 1) * 4096, which * D:(which + 1) * D],
                        in_=src[rr * 4096:(rr + 1) * 4096, :]), pw=[uvb[l]])

        convert_tables(0)

        def rstd_of(src, n_feat, ms, rstd, junk, src_ap=None):
            sap = src.t[:, :] if src_ap is None else src_ap
            k.op('act', lambda e: e.activation(out=junk.t[:, :], in_=sap, func=AF.Square,
                                               scale=float(n_feat) ** -0.5, accum_out=ms.t[:, :]),
                 r=[src], w=[junk, ms])
            k.op('act', lambda e: e.activation(out=ms.t[:, :], in_=ms.t[:, :], func=AF.Ln,
                                               bias=epsb.t[:, :], scale=1.0), r=[epsb], w=[ms])
            k.op('act', lambda e: e.activation(out=rstd.t[:, :], in_=ms.t[:, :], func=AF.Exp, scale=-0.5),
                 r=[ms], w=[rstd])

        class PeerState:
            pass

        def peer_alloc(pes):
            st = PeerState()
            st.iota = k.sb(pes, "iota", [128, 2048], F32)
            k.dma('sp', 'iota', lambda e: e.dma_start(out=st.iota.t[:, :], in_=iota_d[:, :]), w=[st.iota])
            st.wq = k.sb(pes, "wq_sb", [128, 8, 2048], BF16)
            st.keysT = k.sb(pes, "keysT_sb", [128, 2048], BF16)
            st.junk = k.sb(pes, "pjunk", [128, D], F32)
            st.ms = k.sb(pes, "pms", [128, 1], F32)
            st.rstd = k.sb(pes, "prstd", [128, 1], F32)
            st.hnb = [k.sb(pes, "hnb%d" % i, [128, D], BF16) for i in range(2)]
            st.hnT = k.sb(pes, "hnT", [128, D], BF16)
            st.qT = k.sb(pes, "qT", [128, 2048], BF16)
            st.scr = k.sb(pes, "scr", [128, 2048], F32)
            st.top = k.sb(pes, "top", [128, 256], F32)
            st.idx = k.sb(pes, "idx", [128, 256], U32)
            st.idxf = k.sb(pes, "idxf", [128, 256], F32)
            st.cand = k.sb(pes, "cand", [128, 2048], F32)
            st.best = k.sb(pes, "best", [128, 128], F32)
            st.pos = k.sb(pes, "pos", [128, 128], U32)
            st.posi = k.sb(pes, "posi", [128, 128], U32)
            st.posj = k.sb(pes, "posj", [128, 128], U32)
            st.pif = k.sb(pes, "pif", [128, 128], F32)
            st.pjf = k.sb(pes, "pjf", [128, 128], F32)
            st.oh = st.scr
            st.ea = k.sb(pes, "ea", [128, 128], F32)
            st.eb = k.sb(pes, "eb", [128, 128], F32)
            st.ef = k.sb(pes, "ef", [128, 128], F32)
            st.ssum = k.sb(pes, "ssum", [128, 8], F32)
            st.gate = [k.sb(pes, "gate%d" % i, [128, 128], F32) for i in range(2)]
            st.eidx = [k.sb(pes, "eidx%d" % i, [128, 128], U32) for i in range(2)]
            st.act = [k.sb(pes, "pact%d" % i, [128, 1], F32) for i in range(4)]
            st.gel = [k.sb(pes, "pgel%d" % i, [128, 1], F32) for i in range(4)]
            st.prod = [k.sb(pes, "prod%d" % i, [128, D], BF16) for i in range(3)]
            st.slots = [k.sb(pes, "gs%d" % i, [128, 2 * D], BF16) for i in range(NB)]
            st.wv = [k.sb(pes, "pwv%d" % i, [128, 1], F32) for i in range(4)]
            st.vs = [k.sb(pes, "pvs%d" % i, [128, D], BF16) for i in range(3)]
            st.gcount = 0
            return st

        def peer_load_weights(st, l):
            for c in range(8):
                k.dma('pool', 'wq%d' % (c % 2),
                      lambda e, c=c: e.dma_start(out=st.wq.t[:, c, :], in_=wq_d[l, c * 128:(c + 1) * 128, :]),
                      pw=[st.wq])
            k.dma('pool', 'keysT', lambda e: e.dma_start(out=st.keysT.t[:, :], in_=keysT_d[l, :, :]),
                  w=[st.keysT])

        def peer_prep(st, h, gain_ap, gain_buf, par):
            hnb, gate, eidx = st.hnb[par], st.gate[par], st.eidx[par]
            rstd_of(h, D, st.ms, st.rstd, st.junk)
            k.op('dve', lambda e: e.scalar_tensor_tensor(out=hnb.t[:, :], in0=h.t[:, :], scalar=st.rstd.t[:, :],
                                                         in1=gain_ap, op0=ALU.mult, op1=ALU.mult),
                 r=[h, st.rstd, gain_buf], w=[hnb])
            pT = ps[0]
            pTb = pT.t[:, :].bitcast(BF16)
            for c in range(8):
                k.op('pe', lambda e, c=c: e.transpose(out=pTb[:, c * 128:(c + 1) * 128],
                                                      in_=hnb.t[:, c * 128:(c + 1) * 128],
                                                      identity=ident_b.t[:, :]),
                     r=[hnb, ident_b], **({'w': [pT]} if c == 0 else {'pw': [pT]}))
            k.op('act', lambda e: e.activation(out=st.hnT.t[:, :], in_=pTb, func=AF.Copy), r=[pT], w=[st.hnT])
            for bnk in range(4):
                pq = ps[1 + bnk % 2]
                for jj in range(4):
                    j = bnk * 4 + jj
                    for c in range(8):
                        first = (jj == 0 and c == 0)
                        k.op('pe', lambda e, j=j, jj=jj, c=c, pq=pq: e.matmul(
                            pq.t[:, jj * 128:(jj + 1) * 128],
                            lhsT=st.wq.t[:, c, j * 128:(j + 1) * 128],
                            rhs=st.hnT.t[:, c * 128:(c + 1) * 128],
                            start=(c == 0), stop=(c == 7)),
                             r=[st.wq, st.hnT], **({'w': [pq]} if first else {'pw': [pq]}))
                k.op('act', lambda e, bnk=bnk, pq=pq: e.activation(
                    out=st.qT.t[:, bnk * 512:(bnk + 1) * 512], in_=pq.t[:, :], func=AF.Copy),
                     r=[pq], **({'w': [st.qT]} if bnk == 0 else {'pw': [st.qT]}))

            def scores(bnk):
                pq = ps[1 + bnk % 2]
                for jj in range(4):
                    g = bnk * 4 + jj
                    k.op('pe', lambda e, g=g, jj=jj, pq=pq: e.matmul(
                        pq.t[:, jj * 128:(jj + 1) * 128],
                        lhsT=st.qT.t[:, g * 128:(g + 1) * 128],
                        rhs=st.keysT.t[:, g * 128:(g + 1) * 128],
                        start=True, stop=True),
                         r=[st.qT, st.keysT], **({'w': [pq]} if jj == 0 else {'pw': [pq]}))

            def topk1(bnk):
                pq = ps[1 + bnk % 2]
                for g in range(bnk * 4, bnk * 4 + 4):
                    sg = pq.t[:, (g % 4) * 128:(g % 4 + 1) * 128]
                    t0 = st.top.t[:, g * 16:g * 16 + 8]
                    t1 = st.top.t[:, g * 16 + 8:g * 16 + 16]
                    scr = st.scr.t[:, g * 128:(g + 1) * 128]
                    k.op('dve', lambda e, sg=sg, t0=t0: e.max(out=t0, in_=sg), r=[pq], pw=[st.top])
                    k.op('dve', lambda e, sg=sg, t0=t0, scr=scr: e.match_replace(
                        out=scr, in_to_replace=t0, in_values=sg, imm_value=NEG), r=[pq, st.top], pw=[st.scr])
                    k.op('dve', lambda e, t1=t1, scr=scr: e.max(out=t1, in_=scr), r=[st.scr], pw=[st.top])
                    k.op('dve', lambda e, g=g, sg=sg, t0=t0: e.max_index(
                        out=st.idx.t[:, g * 16:g * 16 + 8], in_max=t0, in_values=sg), r=[pq, st.top], pw=[st.idx])
                    k.op('dve', lambda e, g=g, sg=sg, t1=t1: e.max_index(
                        out=st.idx.t[:, g * 16 + 8:g * 16 + 16], in_max=t1, in_values=sg), r=[pq, st.top], pw=[st.idx])

            scores(0)
            scores(1)
            topk1(0)
            scores(2)
            topk1(1)
            scores(3)
            topk1(2)
            topk1(3)
            k.op('dve', lambda e: e.tensor_copy(out=st.idxf.t[:, :], in_=st.idx.t[:, :]), r=[st.idx], w=[st.idxf])
            top4 = st.top.t[:, :].rearrange("p (h two i) -> p h two i", h=8, two=2)
            in0 = top4[:, :, 0, :].unsqueeze(3).to_broadcast([128, 8, 16, 16])
            in1 = top4[:, :, 1, :].unsqueeze(2).to_broadcast([128, 8, 16, 16])
            cand4 = st.cand.t[:, :].rearrange("p (h i j) -> p h i j", h=8, i=16)
            k.op('dve', lambda e: e.tensor_tensor(out=cand4, in0=in0, in1=in1, op=ALU.add), r=[st.top], w=[st.cand])
            for hh in range(8):
                cg = st.cand.t[:, hh * 256:(hh + 1) * 256]
                b0 = st.best.t[:, hh * 16:hh * 16 + 8]
                b1 = st.best.t[:, hh * 16 + 8:hh * 16 + 16]
                scr = st.scr.t[:, hh * 256:(hh + 1) * 256]
                k.op('dve', lambda e, cg=cg, b0=b0: e.max(out=b0, in_=cg), r=[st.cand], pw=[st.best])
                k.op('dve', lambda e, cg=cg, b0=b0, scr=scr: e.match_replace(
                    out=scr, in_to_replace=b0, in_values=cg, imm_value=NEG), r=[st.cand, st.best], pw=[st.scr])
                k.op('dve', lambda e, b1=b1, scr=scr: e.max(out=b1, in_=scr), r=[st.scr], pw=[st.best])
                k.op('dve', lambda e, hh=hh, cg=cg, b0=b0: e.max_index(
                    out=st.pos.t[:, hh * 16:hh * 16 + 8], in_max=b0, in_values=cg), r=[st.cand, st.best], pw=[st.pos])
                k.op('dve', lambda e, hh=hh, cg=cg, b1=b1: e.max_index(
                    out=st.pos.t[:, hh * 16 + 8:hh * 16 + 16], in_max=b1, in_values=cg), r=[st.cand, st.best], pw=[st.pos])
            best3 = st.best.t[:, :].rearrange("p (h k) -> p h k", h=8)
            gate3 = gate.t[:, :].rearrange("p (h k) -> p h k", h=8)
            k.op('dve', lambda e: e.tensor_tensor(out=gate3, in0=best3,
                                                  in1=best3[:, :, 0:1].to_broadcast([128, 8, 16]), op=ALU.subtract),
                 r=[st.best], w=[gate])
            k.op('act', lambda e: e.activation(out=gate.t[:, :], in_=gate.t[:, :], func=AF.Exp), r=[gate], w=[gate])
            k.op('dve', lambda e: e.tensor_reduce(out=st.ssum.t[:, :], in_=gate3, axis=AX.X, op=ALU.add),
                 r=[gate], w=[st.ssum])
            k.op('dve', lambda e: e.reciprocal(out=st.ssum.t[:, :], in_=st.ssum.t[:, :]), r=[st.ssum], w=[st.ssum])
            k.op('dve', lambda e: e.tensor_tensor(out=gate3, in0=gate3,
                                                  in1=st.ssum.t[:, :].unsqueeze(2).to_broadcast([128, 8, 16]),
                                                  op=ALU.mult), r=[gate, st.ssum], w=[gate])
            k.op('dve', lambda e: e.tensor_scalar(out=st.posi.t[:, :], in0=st.pos.t[:, :], scalar1=4, scalar2=None,
                                                  op0=ALU.logical_shift_right), r=[st.pos], w=[st.posi])
            k.op('dve', lambda e: e.tensor_scalar(out=st.posj.t[:, :], in0=st.pos.t[:, :], scalar1=15, scalar2=None,
                                                  op0=ALU.bitwise_and), r=[st.pos], w=[st.posj])
            k.op('dve', lambda e: e.tensor_copy(out=st.pif.t[:, :], in_=st.posi.t[:, :]), r=[st.posi], w=[st.pif])
            k.op('dve', lambda e: e.tensor_copy(out=st.pjf.t[:, :], in_=st.posj.t[:, :]), r=[st.posj], w=[st.pjf])
            iota4 = st.iota.t[:, :].rearrange("p (h k i) -> p h k i", h=8, k=16)
            oh4 = st.oh.t[:, :].rearrange("p (h k i) -> p h k i", h=8, k=16)
            idx4 = st.idxf.t[:, :].rearrange("p (h two i) -> p h two i", h=8, two=2)
            for which, pf, dst in ((0, st.pif, st.ea), (1, st.pjf, st.eb)):
                pf4 = pf.t[:, :].rearrange("p (h k) -> p h k", h=8).unsqueeze(3).to_broadcast([128, 8, 16, 16])
                k.op('dve', lambda e, pf4=pf4: e.tensor_tensor(out=oh4, in0=iota4, in1=pf4, op=ALU.is_equal),
                     r=[st.iota, pf], w=[st.oh])
                ix = idx4[:, :, which, :].unsqueeze(2).to_broadcast([128, 8, 16, 16])
                k.op('dve', lambda e, ix=ix: e.tensor_tensor(out=oh4, in0=oh4, in1=ix, op=ALU.mult),
                     r=[st.oh, st.idxf], w=[st.oh])
                k.op('dve', lambda e, dst=dst: e.tensor_reduce(
                    out=dst.t[:, :], in_=st.oh.t[:, :].rearrange("p (a i) -> p a i", i=16), axis=AX.X, op=ALU.add),
                     r=[st.oh], w=[dst])
            k.op('dve', lambda e: e.scalar_tensor_tensor(out=st.ef.t[:, :], in0=st.ea.t[:, :], scalar=128.0,
                                                         in1=st.eb.t[:, :], op0=ALU.mult, op1=ALU.add),
                 r=[st.ea, st.eb], w=[st.ef])
            k.op('dve', lambda e: e.tensor_copy(out=eidx.t[:, :], in_=st.ef.t[:, :]), r=[st.ef], w=[eidx])

        class Pipe:
            def __init__(self):
                self.n = 0
                self.sched = {}

            def add(self, step, fn):
                self.sched.setdefault(step, []).append(fn)

            def run(self, step):
                for fn in self.sched.pop(step, []):
                    fn()

            def flush(self):
                for step in sorted(self.sched):
                    self.run(step)

        def peer_gather(pipe, st, l, h, par, ybanks, rec=None, fin=None):
            hnb, gate, eidx = st.hnb[par], st.gate[par], st.eidx[par]
            pa, pb = ybanks
            slot_of = {}

            def G(s_):
                slot = st.slots[st.gcount % NB]
                st.gcount += 1
                slot_of[s_] = slot
                k.dma('pool', 'g_' + slot.name, lambda e: e.indirect_dma_start(
                    out=slot.t[:, :], out_offset=None, in_=UVb[l][:, :],
                    in_offset=bass.IndirectOffsetOnAxis(ap=eidx.t[:, s_:s_ + 1], axis=0)),
                      r=[eidx, uvb[l]], w=[slot])

            def Dt(s_):
                slot = slot_of[s_]
                pr = st.prod[s_ % 3]
                k.op('dve', lambda e: e.tensor_tensor(out=pr.t[:, :], in0=slot.t[:, 0:D], in1=hnb.t[:, :], op=ALU.mult),
                     r=[slot, hnb], w=[pr])

            def P(s_):
                pr = st.prod[s_ % 3]
                pd = ps[3 + s_ % 2]
                for c in range(8):
                    k.op('pe', lambda e, c=c: e.matmul(
                        pd.t[:, 0:128], lhsT=ident_b.t[:, :], rhs=pr.t[:, c * 128:(c + 1) * 128],
                        start=(c == 0), stop=(c == 7)),
                         r=[ident_b, pr], **({'w': [pd]} if c == 0 else {'pw': [pd]}))

            def R(s_):
                pd = ps[3 + s_ % 2]
                a_ = st.act[s_ % 4]
                k.op('dve', lambda e: e.tensor_reduce(out=a_.t[:, :], in_=pd.t[:, 0:128], axis=AX.X, op=ALU.add),
                     r=[pd], w=[a_])

            def E(s_):
                a_, g_ = st.act[s_ % 4], st.gel[s_ % 4]
                k.op('act', lambda e: e.activation(out=g_.t[:, :], in_=a_.t[:, :], func=AF.Gelu), r=[a_], w=[g_])

            def X(s_):
                g_, w_ = st.gel[s_ % 4], st.wv[s_ % 4]
                k.op('dve', lambda e: e.tensor_tensor(out=w_.t[:, :], in0=g_.t[:, :], in1=gate.t[:, s_:s_ + 1],
                                                      op=ALU.mult), r=[g_, gate], w=[w_])

            def W(s_):
                slot, w_, vs = slot_of[s_], st.wv[s_ % 4], st.vs[s_ % 3]
                k.op('act', lambda e: e.activation(out=vs.t[:, :], in_=slot.t[:, D:2 * D], func=AF.Copy,
                                                   scale=w_.t[:, :]), r=[slot, w_], w=[vs])

            def M(s_):
                vs = st.vs[s_ % 3]
                for half, pb_ in ((0, pa), (1, pb)):
                    k.op('pe', lambda e, half=half, pb_=pb_: e.matmul(
                        pb_.t[:, :], lhsT=ident_b.t[:, :], rhs=vs.t[:, half * 512:(half + 1) * 512],
                        start=(s_ == 0), stop=(s_ == 127)),
                         r=[ident_b, vs], **({'w': [pb_]} if s_ == 0 else {'pw': [pb_]}))

            def final():
                for half, pb_ in ((0, pa), (1, pb)):
                    k.op('dve', lambda e, half=half, pb_=pb_: e.tensor_tensor(
                        out=h.t[:, half * 512:(half + 1) * 512], in0=h.t[:, half * 512:(half + 1) * 512],
                        in1=pb_.t[:, :], op=ALU.add), r=[pb_, h], pw=[h])
                if fin is not None:
                    fin()

            B = pipe.n
            for s_ in range(128):
                pipe.add(B + s_, lambda s_=s_: G(s_))
                pipe.add(B + s_, lambda s_=s_: Dt(s_))
                pipe.add(B + s_ + 1, lambda s_=s_: P(s_))
                pipe.add(B + s_ + 2, lambda s_=s_: R(s_))
                pipe.add(B + s_ + 3, lambda s_=s_: E(s_))
                pipe.add(B + s_ + 4, lambda s_=s_: X(s_))
                pipe.add(B + s_ + 5, lambda s_=s_: W(s_))
                pipe.add(B + s_ + 6, lambda s_=s_: M(s_))
            pipe.add(B + 127 + 6, final)
            for s_ in range(128):
                pipe.run(B + s_)
                if rec and s_ >= 8:
                    nd_left = sum(1 for r_ in rec if r_[0] == 'op' and r_[1][0] == 'dve')
                    left = max(1, 112 - s_)
                    k.replay(rec, 24, ndve=min(4, max(2, (nd_left + left - 1) // left)), nact=2,
                             chain_break=(s_ < 112))
                if rec and s_ == 124:
                    k.replay(rec, len(rec))
            if rec:
                k.replay(rec, len(rec))
            pipe.n = B + 128

        with ExitStack() as p1:
            gains = k.sb(p1, "gains", [128, 3, D], F32)
            for gi in range(3):
                k.dma('sp', 'gn%d' % gi, lambda e, gi=gi: e.dma_start(
                    out=gains.t[:, gi, :], in_=gains_d[gi:gi + 1, :].partition_broadcast(128)), pw=[gains])
            poolw = k.sb(p1, "poolw", [128, 8, 256], BF16)
            for g in range(4):
                for cc in range(2):
                    k.dma('pool', 'pw%d' % cc, lambda e, g=g, cc=cc: e.dma_start(
                        out=poolw.t[:, g * 2 + cc, :], in_=poolw_d[g, cc * 128:(cc + 1) * 128, :]), pw=[poolw])
            bands = k.sb(p1, "bands", [128, 12, 128], BF16)
            k.dma('pool', 'bands', lambda e: e.dma_start(out=bands.t[:, :, :], in_=bands_d[:, :, :]), w=[bands])
            st = peer_alloc(p1)
            peer_load_weights(st, 0)
            xt = [k.sb(p1, "xt%d" % i, [128, D], F32) for i in range(3)]
            hn = [k.sb(p1, "hn%d" % i, [128, D], BF16) for i in range(2)]
            pooledT = k.sb(p1, "pooledT", [128, D], BF16)
            ms1 = k.sb(p1, "ms1", [128, 1], F32)
            rstd1 = k.sb(p1, "rstd1", [128, 1], F32)
            mixs = k.sb(p1, "mixs", [128, D], F32)

            def load1(i):
                x_ = xt[i % 3]
                k.dma('sp', 'x%d' % (i % 3), lambda e: e.dma_start(out=x_.t[:, :], in_=xpad[i * 128:(i + 1) * 128, :]),
                      w=[x_])

            def front1(i):
                par = i % 2
                x_ = xt[i % 3]
                rstd_of(x_, D, ms1, rstd1, st.junk)
                k.op('dve', lambda e: e.scalar_tensor_tensor(out=hn[par].t[:, :], in0=x_.t[:, :], scalar=rstd1.t[:, :],
                                                             in1=gains.t[:, 0, :], op0=ALU.mult, op1=ALU.mult),
                     r=[x_, rstd1, gains], w=[hn[par]])
                for bnk in range(2):
                    pp = ps[(0, 5)[bnk]]
                    for cc in range(4):
                        c = bnk * 4 + cc
                        g = c // 2
                        bcur = (8 + g) if i == 0 else g
                        k.op('pe', lambda e, c=c, cc=cc, pp=pp, bcur=bcur: e.matmul(
                            pp.t[:, cc * 128:(cc + 1) * 128], lhsT=hn[par].t[:, c * 128:(c + 1) * 128],
                            rhs=bands.t[:, bcur, :], start=True, stop=(i == 0)),
                             r=[hn[par], bands], **({'w': [pp]} if cc == 0 else {'pw': [pp]}))
                        if i > 0:
                            k.op('pe', lambda e, c=c, cc=cc, pp=pp, g=g: e.matmul(
                                pp.t[:, cc * 128:(cc + 1) * 128], lhsT=hn[1 - par].t[:, c * 128:(c + 1) * 128],
                                rhs=bands.t[:, 4 + g, :], start=False, stop=True),
                                 r=[hn[1 - par], bands], pw=[pp])
                    k.op('act', lambda e, bnk=bnk, pp=pp: e.activation(
                        out=pooledT.t[:, bnk * 512:(bnk + 1) * 512], in_=pp.t[:, :], func=AF.Copy),
                         r=[pp], **({'w': [pooledT]} if bnk == 0 else {'pw': [pooledT]}))
                for bnk in range(2):
                    pp = ps[(0, 5)[bnk]]
                    for gg in range(2):
                        g = bnk * 2 + gg
                        for cc in range(2):
                            c = g * 2 + cc
                            k.op('pe', lambda e, c=c, gg=gg, cc=cc, pp=pp: e.matmul(
                                pp.t[:, gg * 256:(gg + 1) * 256], lhsT=pooledT.t[:, c * 128:(c + 1) * 128],
                                rhs=poolw.t[:, c, :], start=(cc == 0), stop=(cc == 1)),
                                 r=[pooledT, poolw], **({'w': [pp]} if (gg == 0 and cc == 0) else {'pw': [pp]}))
                    sl = slice(bnk * 512, (bnk + 1) * 512)
                    k.op('dve', lambda e, pp=pp, sl=sl: e.tensor_tensor(
                        out=mixs.t[:, sl], in0=pp.t[:, :], in1=gains.t[:, 2, sl], op=ALU.mult),
                         r=[pp, gains], **({'w': [mixs]} if bnk == 0 else {'pw': [mixs]}))
                k.op('dve', lambda e: e.tensor_tensor(out=x_.t[:, :], in0=x_.t[:, :], in1=mixs.t[:, :], op=ALU.add),
                     r=[mixs], w=[x_])
                peer_prep(st, x_, gains.t[:, 1, :], gains, par)

            pipe1 = Pipe()

            def back1(i, rec=None):
                par = i % 2
                x_ = xt[i % 3]
                peer_gather(pipe1, st, 0, x_, par, (ps[6], ps[7]), rec,
                            fin=lambda: k.dma('sp', 'h2st%d' % par, lambda e: e.dma_start(
                                out=H2[i * 128:(i + 1) * 128, :], in_=x_.t[:, :]), r=[x_]))

            load1(0)
            if NT > 1:
                load1(1)
            front1(0)
            for i in range(NT):
                k.rec = rec = []
                if i + 2 < NT:
                    load1(i + 2)
                if i + 1 < NT:
                    front1(i + 1)
                k.rec = None
                back1(i, rec)
            pipe1.flush()
            k.barrier()

        with ExitStack() as p2:
            gains2 = k.sb(p2, "gains2", [128, 2, D], F32)
            for gi, src in ((0, 3), (1, 4)):
                k.dma('sp', 'gn%d' % gi, lambda e, gi=gi, src=src: e.dma_start(
                    out=gains2.t[:, gi, :], in_=gains_d[src:src + 1, :].partition_broadcast(128)), pw=[gains2])
            hnm = k.sb(p2, "hnm", [128, 2, 64], F32)
            for gi in range(2):
                k.dma('sp', 'hn%d' % gi, lambda e, gi=gi: e.dma_start(
                    out=hnm.t[:, gi, :], in_=hnorm_d[gi:gi + 1, :].partition_broadcast(128)), pw=[hnm])
            k.op('dve', lambda e: e.tensor_scalar(out=hnm.t[:, 1, :], in0=hnm.t[:, 1, :], scalar1=0.125, scalar2=None,
                                                  op0=ALU.mult), r=[hnm], w=[hnm])
            wkv = k.sb(p2, "wkv", [128, 8, 2048], BF16)
            wqa = k.sb(p2, "wqa", [128, 8, D], BF16)
            for c in range(8):
                k.dma('pool', 'wq%d' % (c % 2), lambda e, c=c: e.dma_start(
                    out=wkv.t[:, c, :], in_=wkv_d[c * 128:(c + 1) * 128, :]), pw=[wkv])
                k.dma('pool', 'pw%d' % (c % 2), lambda e, c=c: e.dma_start(
                    out=wqa.t[:, c, :], in_=wqa_d[c * 128:(c + 1) * 128, :]), pw=[wqa])
            ht = [k.sb(p2, "ht%d" % i, [128, D], F32) for i in range(2)]
            junk2 = k.sb(p2, "junk2", [128, D], F32)
            ms2 = k.sb(p2, "ms2", [128, 1], F32)
            rstd2 = k.sb(p2, "rstd2", [128, 1], F32)
            nb_ = [k.sb(p2, "nb%d" % i, [128, D], BF16) for i in range(2)]
            nT = [k.sb(p2, "nT%d" % i, [128, D], BF16) for i in range(2)]
            sq = k.sb(p2, "sq", [128, D], F32)
            ss = k.sb(p2, "ss", [128, 16], F32)
            tmpn = k.sb(p2, "tmpn", [128, D], F32)
            knb = k.sb(p2, "knb", [128, D], BF16)
            kT = [k.sb(p2, "kT%d" % i, [128, D], BF16) for i in range(2)]
            vb = [k.sb(p2, "vb%d" % i, [128, D], BF16) for i in range(2)]

            def headnorm_T(src_banks, gi, dstT, dram, i, tag):
                for half, pb_ in enumerate(src_banks):
                    sl = slice(half * 512, (half + 1) * 512)
                    k.op('act', lambda e, pb_=pb_, sl=sl: e.activation(out=sq.t[:, sl], in_=pb_.t[:, :], func=AF.Square),
                         r=[pb_], **({'w': [sq]} if half == 0 else {'pw': [sq]}))
                k.op('dve', lambda e: e.tensor_reduce(out=ss.t[:, :], in_=sq.t[:, :].rearrange("p (h d) -> p h d", d=64),
                                                      axis=AX.X, op=ALU.add), r=[sq], w=[ss])
                k.op('act', lambda e: e.activation(out=ss.t[:, :], in_=ss.t[:, :], func=AF.Ln, bias=epsb.t[:, :],
                                                   scale=1.0 / 64), r=[ss, epsb], w=[ss])
                k.op('act', lambda e: e.activation(out=ss.t[:, :], in_=ss.t[:, :], func=AF.Exp, scale=-0.5),
                     r=[ss], w=[ss])
                for half, pb_ in enumerate(src_banks):
                    sl = slice(half * 512, (half + 1) * 512)
                    k.op('dve', lambda e, pb_=pb_, sl=sl, half=half: e.tensor_tensor(
                        out=tmpn.t[:, sl].rearrange("p (h d) -> p h d", d=64),
                        in0=pb_.t[:, :].rearrange("p (h d) -> p h d", d=64),
                        in1=ss.t[:, half * 8:(half + 1) * 8].unsqueeze(2).to_broadcast([128, 8, 64]), op=ALU.mult),
                         r=[pb_, ss], **({'w': [tmpn]} if half == 0 else {'pw': [tmpn]}))
                k.op('dve', lambda e: e.tensor_tensor(
                    out=knb.t[:, :].rearrange("p (h d) -> p h d", d=64),
                    in0=tmpn.t[:, :].rearrange("p (h d) -> p h d", d=64),
                    in1=hnm.t[:, gi, :].unsqueeze(1).to_broadcast([128, 16, 64]), op=ALU.mult),
                     r=[tmpn, hnm], w=[knb])
                pT = src_banks[0]
                pTb = pT.t[:, :].bitcast(BF16)
                for c in range(8):
                    k.op('pe', lambda e, c=c: e.transpose(out=pTb[:, c * 128:(c + 1) * 128],
                                                          in_=knb.t[:, c * 128:(c + 1) * 128], identity=ident_b.t[:, :]),
                         r=[knb, ident_b], **({'w': [pT]} if c == 0 else {'pw': [pT]}))
                k.op('act', lambda e: e.activation(out=dstT.t[:, :], in_=pTb, func=AF.Copy), r=[pT], w=[dstT])
                for hh in range(2):
                    k.dma('sp', '%s%d_%d' % (tag, i % 2, hh), lambda e, hh=hh: e.dma_start(
                        out=dram.rearrange("(pr two) d l -> two d pr l", two=2)[hh, :, :, i * 128:(i + 1) * 128],
                        in_=dstT.t[hh * 64:(hh + 1) * 64, :].rearrange("p (pr t) -> p pr t", t=128)),
                          r=[dstT])

            for i in range(NT):
                par = i % 2
                h_ = ht[par]
                k.dma('sp', 'x%d' % par, lambda e: e.dma_start(out=h_.t[:, :], in_=H2[i * 128:(i + 1) * 128, :]), w=[h_])
                rstd_of(h_, D, ms2, rstd2, junk2)
                for gi in range(2):
                    k.op('dve', lambda e, gi=gi: e.scalar_tensor_tensor(
                        out=nb_[gi].t[:, :], in0=h_.t[:, :], scalar=rstd2.t[:, :], in1=gains2.t[:, gi, :],
                        op0=ALU.mult, op1=ALU.mult), r=[h_, rstd2, gains2], w=[nb_[gi]])
                    pT = ps[gi]
                    pTb = pT.t[:, :].bitcast(BF16)
                    for c in range(8):
                        k.op('pe', lambda e, c=c, gi=gi, pTb=pTb: e.transpose(
                            out=pTb[:, c * 128:(c + 1) * 128], in_=nb_[gi].t[:, c * 128:(c + 1) * 128],
                            identity=ident_b.t[:, :]),
                             r=[nb_[gi], ident_b], **({'w': [pT]} if c == 0 else {'pw': [pT]}))
                    k.op('act', lambda e, gi=gi, pTb=pTb: e.activation(out=nT[gi].t[:, :], in_=pTb, func=AF.Copy),
                         r=[pT], w=[nT[gi]])
                for blk in range(4):
                    pb_ = ps[2 + blk]
                    for c in range(8):
                        k.op('pe', lambda e, c=c, blk=blk, pb_=pb_: e.matmul(
                            pb_.t[:, :], lhsT=nT[0].t[:, c * 128:(c + 1) * 128],
                            rhs=wkv.t[:, c, blk * 512:(blk + 1) * 512], start=(c == 0), stop=(c == 7)),
                             r=[nT[0], wkv], **({'w': [pb_]} if c == 0 else {'pw': [pb_]}))
                for blk in range(2):
                    pb_ = ps[6 + blk]
                    for c in range(8):
                        k.op('pe', lambda e, c=c, blk=blk, pb_=pb_: e.matmul(
                            pb_.t[:, :], lhsT=nT[1].t[:, c * 128:(c + 1) * 128],
                            rhs=wqa.t[:, c, blk * 512:(blk + 1) * 512], start=(c == 0), stop=(c == 7)),
                             r=[nT[1], wqa], **({'w': [pb_]} if c == 0 else {'pw': [pb_]}))
                v_ = vb[par]
                for half in range(2):
                    k.op('act', lambda e, half=half: e.activation(
                        out=v_.t[:, half * 512:(half + 1) * 512], in_=ps[4 + half].t[:, :], func=AF.Copy),
                         r=[ps[4 + half]], **({'w': [v_]} if half == 0 else {'pw': [v_]}))
                k.dma('sp', 'vst%d' % par, lambda e: e.dma_start(out=VV[i * 128:(i + 1) * 128, :], in_=v_.t[:, :]), r=[v_])
                headnorm_T((ps[2], ps[3]), 0, kT[0], KT, i, 'kst')
                headnorm_T((ps[6], ps[7]), 1, kT[1], QT, i, 'qst')
            k.barrier()

        with ExitStack() as p3:
            kth = [k.sb(p3, "kth%d" % i, [64, Lp], BF16) for i in range(2)]
            qth = [k.sb(p3, "qth%d" % i, [64, Lp], BF16) for i in range(2)]
            vh = [k.sb(p3, "vh%d" % i, [128, NT, 64], BF16) for i in range(2)]
            e_sb = [k.sb(p3, "e_sb%d" % i, [128, 512], F32) for i in range(2)]
            sp_sb = [k.sb(p3, "sp_sb%d" % i, [128, 512], BF16) for i in range(3)]
            wg_sb = [k.sb(p3, "wg_sb%d" % i, [128, 512], BF16) for i in range(3)]
            S = [k.sb(p3, "S%d" % i, [128, 512], BF16) for i in range(2)]
            o_sb = [k.sb(p3, "o_sb%d" % i, [64, 512], BF16) for i in range(2)]
            convert_tables(1)
            one = k.sb(p3, "onec", [128, 1], F32)
            k.op('dve', lambda e: e.memset(one.t[:, :], 1.0), w=[one])
            NA = 5
            tasks = []

            def load_head(head):
                hp = head % 2
                k.dma('sp', 'kth%d' % hp, lambda e: e.dma_start(out=kth[hp].t[:, :], in_=KT[head, :, :]), w=[kth[hp]])
                k.dma('sp', 'qth%d' % hp, lambda e: e.dma_start(out=qth[hp].t[:, :], in_=QT[head, :, :]), w=[qth[hp]])
                k.dma('sp', 'vh%d' % hp, lambda e: e.dma_start(
                    out=vh[hp].t[:, :, :],
                    in_=VV.rearrange("(j p) d -> p j d", p=128)[:, :, head * 64:(head + 1) * 64]), w=[vh[hp]])

            def make_task(t, head, qa, qb, j, n, g):
                hp = head % 2
                ncols = (qb - qa + 1) * 128
                cs = max(j - qa, 0) * 128
                A = ps[t % NA]
                eb_ = e_sb[t % 2]
                spb = sp_sb[t % 3]
                wgb = wg_sb[t % 3]
                O = ps[NA + g % 2]
                osb = o_sb[g % 2]
                diag = j >= qa
                first = (n == 0)
                last = (j == 0)
                Sold = S[(n - 1) % 2]
                Snew = S[n % 2]

                def s1():
                    if first and qa == 1 and head + 1 < 16:
                        load_head(head + 1)
                    if first:
                        k.op('dve', lambda e: e.memset(O.t[0:64, :], 0.0), w=[O])
                    k.op('pe', lambda e: e.matmul(
                        A.t[:, cs:ncols], lhsT=kth[hp].t[:, j * 128:(j + 1) * 128],
                        rhs=qth[hp].t[:, qa * 128 + cs:qa * 128 + ncols], start=True, stop=False),
                         r=[kth[hp], qth[hp]], w=[A])

                def s2():
                    k.op('act', lambda e: e.activation(
                        out=eb_.t[:, cs:ncols], in_=A.t[:, cs:ncols], func=AF.Exp), r=[A], w=[eb_])
                    k.op('act', lambda e: e.activation(
                        out=spb.t[:, cs:ncols], in_=eb_.t[:, cs:ncols], func=AF.Ln, bias=one.t[:, :], scale=1.0),
                         r=[eb_, one], w=[spb])
                    if diag:
                        k.op('dve', lambda e: e.tensor_tensor(
                            out=spb.t[:, cs:cs + 128], in0=spb.t[:, cs:cs + 128], in1=trim.t[:, :], op=ALU.mult),
                             r=[spb, trim], w=[spb])
                    if j == 0:
                        k.op('dve', lambda e: e.tensor_scalar(
                            out=spb.t[:, cs:ncols], in0=spb.t[:, cs:ncols], scalar1=rowm.t[:, :], scalar2=None,
                            op0=ALU.mult), r=[spb, rowm], w=[spb])

                def s3():
                    k.op('pe', lambda e: e.matmul(
                        A.t[:, cs:ncols], lhsT=negtri.t[:, :], rhs=spb.t[:, cs:ncols], start=False, stop=first),
                         r=[negtri, spb], pw=[A])
                    if not first:
                        k.op('pe', lambda e: e.matmul(
                            A.t[:, cs:ncols], lhsT=negones.t[:, :], rhs=Sold.t[:, cs:ncols], start=False, stop=True),
                             r=[negones, Sold], pw=[A])
                    if j > 0:
                        if first:
                            k.op('dve', lambda e: e.memset(Snew.t[:, :], 0.0), w=[Snew])
                            k.op('dve', lambda e: e.tensor_copy(
                                out=Snew.t[:, cs:ncols], in_=spb.t[:, cs:ncols]), r=[spb], w=[Snew])
                        elif cs > 0:
                            k.op('dve', lambda e: e.memset(Snew.t[:, 0:cs], 0.0), w=[Snew])
                            k.op('dve', lambda e: e.tensor_tensor(
                                out=Snew.t[:, cs:ncols], in0=Sold.t[:, cs:ncols], in1=spb.t[:, cs:ncols],
                                op=ALU.add), r=[Sold, spb], pw=[Snew])
                        else:
                            k.op('dve', lambda e: e.tensor_tensor(
                                out=Snew.t[:, 0:ncols], in0=Sold.t[:, 0:ncols], in1=spb.t[:, 0:ncols],
                                op=ALU.add), r=[Sold, spb], w=[Snew])

                def s4():
                    k.op('act', lambda e: e.activation(
                        out=wgb.t[:, cs:ncols], in_=A.t[:, cs:ncols], func=AF.Exp), r=[A], w=[wgb])
                    if diag:
                        k.op('dve', lambda e: e.tensor_tensor(
                            out=wgb.t[:, cs:cs + 128], in0=wgb.t[:, cs:cs + 128], in1=trim.t[:, :], op=ALU.mult),
                             r=[wgb, trim], w=[wgb])
                    if j == 0:
                        k.op('dve', lambda e: e.tensor_scalar(
                            out=wgb.t[:, cs:ncols], in0=wgb.t[:, cs:ncols], scalar1=rowm.t[:, :], scalar2=None,
                            op0=ALU.mult), r=[wgb, rowm], w=[wgb])

                def s5():
                    k.op('pe', lambda e: e.matmul(
                        O.t[0:64, cs:ncols], lhsT=vh[hp].t[:, j, :], rhs=wgb.t[:, cs:ncols],
                        start=False, stop=last, skip_group_check=True),
                         r=[vh[hp], wgb], pw=[O])
                    if last:
                        k.op('act', lambda e: e.activation(
                            out=osb.t[:, 0:ncols], in_=O.t[0:64, 0:ncols], func=AF.Copy), r=[O], w=[osb])
                        k.dma('sp', 'ost' + osb.name, lambda e: e.dma_start(
                            out=AT[head * 64:(head + 1) * 64, qa * 128:qa * 128 + ncols], in_=osb.t[:, 0:ncols]),
                              r=[osb])
                return (s1, s2, s3, s4, s5)

            t = 0
            g = 0
            for head in range(16):
                for qa in range(1, NT, 4):
                    qb = min(qa + 3, NT - 1)
                    for n, j in enumerate(range(qb, -1, -1)):
                        tasks.append(make_task(t, head, qa, qb, j, n, g))
                        t += 1
                    g += 1
            load_head(0)
            NTK = len(tasks)
            for step in range(NTK + 4):
                for st_i in (4, 3, 2, 1, 0):
                    ti = step - st_i
                    if 0 <= ti < NTK:
                        tasks[ti][st_i]()
            k.barrier()

        with ExitStack() as p4:
            gains4 = k.sb(p4, "gains4", [128, D], F32)
            k.dma('sp', 'gn0', lambda e: e.dma_start(out=gains4.t[:, :], in_=gains_d[5:6, :].partition_broadcast(128)),
                  w=[gains4])
            wo = k.sb(p4, "wo", [128, 8, D], BF16)
            for c in range(8):
                k.dma('pool', 'pw%d' % (c % 2), lambda e, c=c: e.dma_start(
                    out=wo.t[:, c, :], in_=wo_d[c * 128:(c + 1) * 128, :]), pw=[wo])
            st = peer_alloc(p4)
            peer_load_weights(st, 1)
            h4 = [k.sb(p4, "h4_%d" % i, [128, D], F32) for i in range(3)]
            att = [k.sb(p4, "att%d" % i, [128, 8, 128], BF16) for i in range(3)]

            def load4(i):
                h_, a_ = h4[i % 3], att[i % 3]
                k.dma('sp', 'x%d' % (i % 3), lambda e: e.dma_start(out=h_.t[:, :], in_=H2[i * 128:(i + 1) * 128, :]), w=[h_])
                k.dma('sp', 'att%d' % (i % 3), lambda e: e.dma_start(
                    out=a_.t[:, :, :],
                    in_=AT.rearrange("(c p) l -> p c l", p=128)[:, :, i * 128:(i + 1) * 128]), w=[a_])

            def front4(i):
                par = i % 2
                h_ = h4[i % 3]
                for half in range(2):
                    pb_ = ps[(0, 5)[half]]
                    for c in range(8):
                        k.op('pe', lambda e, c=c, half=half, pb_=pb_: e.matmul(
                            pb_.t[:, :], lhsT=att[i % 3].t[:, c, :], rhs=wo.t[:, c, half * 512:(half + 1) * 512],
                            start=(c == 0), stop=(c == 7)),
                             r=[att[i % 3], wo], **({'w': [pb_]} if c == 0 else {'pw': [pb_]}))
                    sl = slice(half * 512, (half + 1) * 512)
                    k.op('dve', lambda e, pb_=pb_, sl=sl: e.tensor_tensor(
                        out=h_.t[:, sl], in0=h_.t[:, sl], in1=pb_.t[:, :], op=ALU.add), r=[pb_, h_], pw=[h_])
                peer_prep(st, h_, gains4.t[:, :], gains4, par)

            pipe4 = Pipe()

            def back4(i, rec=None):
                par = i % 2
                h_ = h4[i % 3]
                peer_gather(pipe4, st, 1, h_, par, (ps[6], ps[7]), rec,
                            fin=lambda: k.dma('sp', 'h2st%d' % par, lambda e: e.dma_start(
                                out=out_d[(i - 1) * 128:i * 128, :], in_=h_.t[:, :]), r=[h_]))

            load4(1)
            if NT > 2:
                load4(2)
            front4(1)
            for i in range(1, NT):
                k.rec = rec = []
                if i + 2 < NT:
                    load4(i + 2)
                if i + 1 < NT:
                    front4(i + 1)
                k.rec = None
                back4(i, rec)
            pipe4.flush()
            k.barrier()
        print("build: ninst=%d" % k.ninst, {e: k.cnt[e] for e in COMPUTE})
    return nc


def _consts():
    ident = np.eye(128, dtype=np.float32)
    kk = np.arange(128)
    trim = (kk[:, None] < kk[None, :]).astype(np.float32)
    negtri = -(kk[:, None] >= kk[None, :]).astype(np.float32)
    negones = -np.ones((128, 128), np.float32)
    cst = np.stack([ident, trim, negtri, negones], axis=1)
    rowm = (kk >= 112).astype(np.float32)[:, None]
    iota = np.tile(np.arange(16, dtype=np.float32), 128)[None, :].repeat(128, axis=0)
    bands = np.zeros((128, 12, 128), np.float32)
    for g, w in enumerate((2, 4, 8, 16)):
        for t in range(128):
            for tp in range(t - w + 1, t + 1):
                if tp >= 0:
                    bands[tp, g, t] += 1.0 / w
                else:
                    bands[128 + tp, 4 + g, t] += 1.0 / w
            bands[t, g, t] -= 1.0
            l = t - 112
            c = float(min(max(l, 0) + 1, w))
            for tp in range(t - w + 1, t + 1):
                if tp >= 0:
                    bands[tp, 8 + g, t] += 1.0 / c
            bands[t, 8 + g, t] -= 1.0
    return cst, rowm, iota, bands


def _in_maps(inputs, NT, cores):
    x = np.asarray(inputs["x"], np.float32)
    meta = np.asarray(inputs["meta_tokens"], np.float32)
    Lp = NT * 128
    cst, rowm, iota, bands = _consts()
    gains = np.stack([inputs["norm_mix"][0], inputs["norm_ffn"][0], inputs["pool_scale"][0], inputs["kv_norm"],
                      inputs["norm_mix"][1], inputs["norm_ffn"][1]]).astype(np.float32)
    hnorm = np.stack([inputs["k_norm"], inputs["q_norm"][0]]).astype(np.float32)
    keysT = np.ascontiguousarray(np.transpose(np.asarray(inputs["peer_keys"], np.float32), (0, 4, 1, 2, 3))
                                 ).reshape(2, 128, 2048)
    shared = {
        "gains": np.ascontiguousarray(gains), "hnorm": np.ascontiguousarray(hnorm),
        "poolw": np.ascontiguousarray(inputs["pool_w"][0], dtype=np.float32), "bands": bands,
        "wq": np.ascontiguousarray(inputs["peer_wq"], dtype=np.float32), "keysT": keysT,
        "wkv": np.ascontiguousarray(inputs["w_kv"], dtype=np.float32),
        "wqa": np.ascontiguousarray(inputs["w_q"][0], dtype=np.float32),
        "wo": np.ascontiguousarray(inputs["w_o"][0], dtype=np.float32),
        "pu0": np.ascontiguousarray(inputs["peer_u"][0], dtype=np.float32),
        "pu1": np.ascontiguousarray(inputs["peer_u"][1], dtype=np.float32),
        "pv0": np.ascontiguousarray(inputs["peer_v"][0], dtype=np.float32),
        "pv1": np.ascontiguousarray(inputs["peer_v"][1], dtype=np.float32),
        "cst": cst, "rowm": rowm, "iota": iota,
    }
    maps = []
    for b in cores:
        xp = np.zeros((Lp, D), np.float32)
        xp[112:128] = meta
        xp[128:] = x[b, :Lp - 128]
        m = dict(shared)
        m["xpad"] = xp
        maps.append(m)
    return maps


def run(inputs, NT, cores, dbg=False):
    nc = build(NT, dbg=dbg)
    maps = _in_maps(inputs, NT, cores)
    res = run_bass_kernel_spmd(nc, maps, core_ids=list(range(len(cores))))
    return res


def kernel(**inputs):
    NT = 65
    res = run(inputs, NT, list(range(8)))
    return np.stack([np.asarray(r["out"], np.float32) for r in res.results], axis=0)
```
